# Optimizing a Trainium2 kernel written in Bass

```python
import math
import jax
import jax.numpy as jnp
from jax import lax
import numpy as np

D_MODEL = 1024
BATCH = 16
SEQ = 2048
DEPTH = 4

N_MIXERS = 4
EPS = 1e-6
F32 = jnp.float32
OUT_SCALE = 0.5
A_HEADS = 4
A_DQK = 128
A_DV = 256
A_CHUNK = 64
A_IN = 2 * A_HEADS * A_DQK + 2 * A_HEADS * A_DV + 2 * A_HEADS
B_HEADS = 8
B_DH = 128
B_BLOCK = 256
B_TOPK = 3
B_QBLOCK = 128
ROPE_THETA = 500000.0
ROPE_DIMS = B_DH // 4
B_IN = 3 * B_HEADS * B_DH
C_HEADS = 4
C_DK = D_MODEL // C_HEADS
C_DV = 2 * C_DK
C_CHUNK = 128
C_ROPE_THETA = 10000.0
C_IN = 2 * C_HEADS * C_DK + 2 * C_HEADS * C_DV
D_RNN = D_MODEL
D_BLOCKS = 4
D_BW = D_RNN // D_BLOCKS
D_CONV = 4
LRU_C = 8.0
D_FF = ((8 * D_MODEL // 3 + 255) // 256) * 256
N_LAYERS_A = len(range(0, DEPTH, N_MIXERS))
N_LAYERS_B = len(range(1, DEPTH, N_MIXERS))
N_LAYERS_C = len(range(2, DEPTH, N_MIXERS))
N_LAYERS_D = len(range(3, DEPTH, N_MIXERS))

kernel_name = 'interleaved_mlstm_moba_retention_rglru_trunk'


def rmsnorm(x, g):
    xf = x.astype(F32)
    y = xf * lax.rsqrt(jnp.mean(xf * xf, axis=-1, keepdims=True) + EPS)
    return (y * g.astype(F32)).astype(x.dtype)


def rotary(x, pos, n_rot, theta):
    half = n_rot // 2
    inv = theta ** (-jnp.arange(half, dtype=F32) * (2.0 / n_rot))
    ang = pos.astype(F32)[:, None, :, None] * inv
    cos, sin = jnp.cos(ang), jnp.sin(ang)
    xf = x.astype(F32)
    x1, x2 = xf[..., :half], xf[..., half:n_rot]
    out = jnp.concatenate([x1 * cos - x2 * sin, x2 * cos + x1 * sin, xf[..., n_rot:]], axis=-1)
    return out.astype(x.dtype)


def split_heads(t, h):
    b, s, _ = t.shape
    return t.reshape(b, s, h, -1).transpose(0, 2, 1, 3)


def merge_heads(t):
    b, h, s, d = t.shape
    return t.transpose(0, 2, 1, 3).reshape(b, s, h * d)


def to_chunks(t, l):
    b, h, s, d = t.shape
    return t.reshape(b, h, s // l, l, d).transpose(2, 0, 1, 3, 4)


def from_chunks(t):
    nc, b, h, l, d = t.shape
    return t.transpose(1, 2, 0, 3, 4).reshape(b, h, nc * l, d)


def mlstm_mixer(u, w_in, b_gates, g_out, w_out):
    b, s, _ = u.shape
    h, l = A_HEADS, A_CHUNK
    nc = s // l
    sq, sv = h * A_DQK, h * A_DV
    q, k, v, o, gl = jnp.split(u @ w_in, [sq, 2 * sq, 2 * sq + sv, 2 * sq + 2 * sv], axis=-1)
    gl = gl.astype(F32) + b_gates.astype(F32)
    log_i = gl[..., :h]
    log_f = jax.nn.log_sigmoid(gl[..., h:])
    qh = split_heads(q, h).astype(F32) * (A_DQK ** -0.5)
    kh = split_heads(k, h).astype(F32)
    vh = split_heads(v, h).astype(F32)

    def gate_chunks(g):
        return g.transpose(0, 2, 1).reshape(b, h, nc, l).transpose(2, 0, 1, 3)

    causal = jnp.tril(jnp.ones((l, l), dtype=bool))

    def step(carry, xs):
        c_st, n_st, m_st = carry
        qc, kc, vc, ic, fc = xs
        bcum = jnp.cumsum(fc, axis=-1)
        btot = bcum[..., -1]
        dmat = jnp.where(causal, bcum[..., :, None] - bcum[..., None, :] + ic[..., None, :], -jnp.inf)
        inter = bcum + m_st[..., None]
        m_t = jnp.maximum(inter, jnp.max(dmat, axis=-1))
        w_intra = jnp.exp(dmat - m_t[..., None])
        w_inter = jnp.exp(inter - m_t)
        qk = jnp.einsum('bhtd,bhsd->bhts', qc, kc) * w_intra
        num = jnp.einsum('bhts,bhsv->bhtv', qk, vc) + w_inter[..., None] * jnp.einsum('bhvd,bhtd->bhtv', c_st, qc)
        den = jnp.sum(qk, axis=-1) + w_inter * jnp.einsum('bhd,bhtd->bht', n_st, qc)
        h_out = num / jnp.maximum(jnp.abs(den), jnp.exp(-m_t))[..., None]
        dec = btot[..., None] - bcum + ic
        m_new = jnp.maximum(btot + m_st, jnp.max(dec, axis=-1))
        ws = jnp.exp(dec - m_new[..., None])
        wc = jnp.exp(btot + m_st - m_new)
        c_new = wc[..., None, None] * c_st + jnp.einsum('bhs,bhsv,bhsd->bhvd', ws, vc, kc)
        n_new = wc[..., None] * n_st + jnp.einsum('bhs,bhsd->bhd', ws, kc)
        return (c_new, n_new, m_new), h_out

    init = (jnp.zeros((b, h, A_DV, A_DQK), F32), jnp.zeros((b, h, A_DQK), F32), jnp.zeros((b, h), F32))
    xs = (to_chunks(qh, l), to_chunks(kh, l), to_chunks(vh, l), gate_chunks(log_i), gate_chunks(log_f))
    _, hc = lax.scan(step, init, xs)
    hs = rmsnorm(from_chunks(hc), g_out[:, None, :])
    y = merge_heads(hs).astype(u.dtype) * jax.nn.sigmoid(o)
    return y @ w_out


def moba_mixer(u, positions, w_in, g_q, g_k, w_out):
    b, s, _ = u.shape
    h, dh = B_HEADS, B_DH
    q, k, v = jnp.split(u @ w_in, 3, axis=-1)
    qh = rotary(rmsnorm(split_heads(q, h), g_q), positions, ROPE_DIMS, ROPE_THETA)
    kh = rotary(rmsnorm(split_heads(k, h), g_k), positions, ROPE_DIMS, ROPE_THETA)
    vh = split_heads(v, h)
    nblk = -(-s // B_BLOCK)
    sp = nblk * B_BLOCK
    pad = ((0, 0), (0, 0), (0, sp - s), (0, 0))
    qh, kh, vh = jnp.pad(qh, pad), jnp.pad(kh, pad), jnp.pad(vh, pad)
    kb = kh.reshape(b, h, nblk, B_BLOCK, dh)
    vb = vh.reshape(b, h, nblk, B_BLOCK, dh)
    kmean = jnp.mean(kb.astype(F32), axis=3)
    nqb = sp // B_QBLOCK
    per_blk = B_BLOCK // B_QBLOCK
    topk = min(B_TOPK, nblk - 1)
    qblocks = qh.reshape(b, h, nqb, B_QBLOCK, dh).transpose(0, 2, 1, 3, 4).reshape(b * nqb, h, B_QBLOCK, dh)
    scale = dh ** -0.5
    qoff = jnp.arange(B_QBLOCK)
    koff = jnp.arange(B_BLOCK)

    def attend(args):
        idx, qc = args
        bi = idx // nqb
        qi = idx % nqb
        j = qi // per_blk
        kb_b, vb_b = kb[bi], vb[bi]
        qf = qc.astype(F32) * scale
        qpos = qi * B_QBLOCK + qoff
        kpos = j * B_BLOCK + koff
        k_own = kb_b[:, j].astype(F32)
        v_own = vb_b[:, j].astype(F32)
        s_own = jnp.einsum('hqd,hnd->hqn', qf, k_own)
        s_own = jnp.where(kpos[None, None, :] <= qpos[None, :, None], s_own, -jnp.inf)
        if topk > 0:
            gate = jnp.einsum('hqd,hnd->hqn', qf, kmean[bi])
            gate = jnp.where(jnp.arange(nblk)[None, None, :] < j, gate, -jnp.inf)
            _, sel = lax.top_k(gate, topk)
            k_sel = jax.vmap(lambda kh_, ih_: kh_[ih_])(kb_b, sel).astype(F32)
            v_sel = jax.vmap(lambda vh_, ih_: vh_[ih_])(vb_b, sel).astype(F32)
            s_sel = jnp.einsum('hqd,hqtnd->hqtn', qf, k_sel)
            valid = jnp.arange(topk) < j
            s_sel = jnp.where(valid[None, None, :, None], s_sel, -jnp.inf)
            logits = jnp.concatenate([s_sel.reshape(h, B_QBLOCK, topk * B_BLOCK), s_own], axis=-1)
            p = jax.nn.softmax(logits, axis=-1)
            p_sel = p[..., :topk * B_BLOCK].reshape(h, B_QBLOCK, topk, B_BLOCK)
            out = jnp.einsum('hqtn,hqtnd->hqd', p_sel, v_sel) + jnp.einsum('hqn,hnd->hqd', p[..., topk * B_BLOCK:], v_own)
        else:
            p = jax.nn.softmax(s_own, axis=-1)
            out = jnp.einsum('hqn,hnd->hqd', p, v_own)
        return out.astype(qc.dtype)

    out = lax.map(attend, (jnp.arange(b * nqb, dtype=jnp.int32), qblocks))
    out = out.reshape(b, nqb, h, B_QBLOCK, dh).transpose(0, 2, 1, 3, 4).reshape(b, h, sp, dh)[:, :, :s]
    return merge_heads(out) @ w_out


def retention_mixer(u, positions, w_in, g_out, w_out):
    b, s, _ = u.shape
    h, l = C_HEADS, C_CHUNK
    sk, sv = h * C_DK, h * C_DV
    q, k, v, g = jnp.split(u @ w_in, [sk, 2 * sk, 2 * sk + sv], axis=-1)
    qh = rotary(split_heads(q, h), positions, C_DK, C_ROPE_THETA).astype(F32)
    kh = rotary(split_heads(k, h), positions, C_DK, C_ROPE_THETA).astype(F32) * (C_DK ** -0.5)
    vh = split_heads(v, h).astype(F32)
    log_g = jnp.log1p(-jnp.exp2(-5.0 - jnp.arange(h, dtype=F32)))
    idx = jnp.arange(l, dtype=F32)
    diff = idx[:, None] - idx[None, :]
    intra = jnp.where(diff >= 0, jnp.exp(jnp.maximum(diff, 0.0)[None] * log_g[:, None, None]), 0.0)
    xi = jnp.exp((idx + 1.0)[None, :] * log_g[:, None])
    zeta = jnp.exp((l - 1.0 - idx)[None, :] * log_g[:, None])
    g_chunk = jnp.exp(l * log_g)

    def step(r_st, xs):
        qc, kc, vc = xs
        inner = jnp.einsum('bhts,bhsv->bhtv', jnp.einsum('bhtd,bhsd->bhts', qc, kc) * intra, vc)
        cross = jnp.einsum('bhtd,bhdv->bhtv', qc, r_st) * xi[None, :, :, None]
        r_new = g_chunk[None, :, None, None] * r_st + jnp.einsum('bhsd,bhsv->bhdv', kc * zeta[None, :, :, None], vc)
        return r_new, inner + cross

    _, yc = lax.scan(step, jnp.zeros((b, h, C_DK, C_DV), F32), (to_chunks(qh, l), to_chunks(kh, l), to_chunks(vh, l)))
    ys = from_chunks(yc)
    mu = jnp.mean(ys, axis=-1, keepdims=True)
    var = jnp.mean(jnp.square(ys - mu), axis=-1, keepdims=True)
    yn = (ys - mu) * lax.rsqrt(var + EPS) * g_out[:, None, :].astype(F32)
    y = merge_heads(yn).astype(u.dtype) * jax.nn.silu(g)
    return y @ w_out


def lru_combine(c1, c2):
    a1, b1 = c1
    a2, b2 = c2
    return a1 * a2, a2 * b1 + b2


def rglru_mixer(u, w_in, conv_w, conv_b, w_gates, b_gates, lru_param, w_out):
    b, s, _ = u.shape
    gate_br, xb = jnp.split(u @ w_in, 2, axis=-1)
    xc = lax.conv_general_dilated(xb, conv_w[:, None, :].astype(xb.dtype), window_strides=(1,),
                                  padding=[(D_CONV - 1, 0)], dimension_numbers=('NWC', 'WIO', 'NWC'),
                                  feature_group_count=D_RNN) + conv_b
    gates = jnp.einsum('bsnc,nce->bsne', xc.reshape(b, s, D_BLOCKS, D_BW), w_gates) + b_gates
    r = jax.nn.sigmoid(gates[..., :D_BW].astype(F32)).reshape(b, s, D_RNN)
    i = jax.nn.sigmoid(gates[..., D_BW:].astype(F32)).reshape(b, s, D_RNN)
    log_a = -LRU_C * r * jax.nn.softplus(-lru_param.astype(F32))
    a = jnp.exp(log_a)
    bterm = jnp.sqrt(-jnp.expm1(2.0 * log_a)) * (i * xc.astype(F32))
    _, hs = lax.associative_scan(lru_combine, (a, bterm), axis=1)
    y = hs.astype(u.dtype) * jax.nn.gelu(gate_br)
    return y @ w_out


def swiglu(u, w_in, w_out):
    gt, up = jnp.split(u @ w_in, 2, axis=-1)
    return (jax.nn.silu(gt) * up) @ w_out


def setup_inputs(seed: int = 0) -> dict:
    key = jax.random.key(seed)
    ks = list(jax.random.split(key, 32))

    def rnd(shape, scale):
        return scale * jax.random.normal(ks.pop(), shape, F32)

    def gain(shape):
        return 1.0 + rnd(shape, 0.02)

    x = rnd((BATCH, SEQ, D_MODEL), 1.0)
    offs = jax.random.randint(ks.pop(), (BATCH, 1), 0, 1024, dtype=jnp.int32)
    positions = jnp.arange(SEQ, dtype=jnp.int32)[None, :] + offs
    norm_mix = gain((DEPTH, D_MODEL))
    norm_ffn = gain((DEPTH, D_MODEL))
    ffn_w_in = rnd((DEPTH, D_MODEL, 2 * D_FF), D_MODEL ** -0.5)
    ffn_w_out = rnd((DEPTH, D_FF, D_MODEL), OUT_SCALE * D_FF ** -0.5)
    a_w_in = rnd((N_LAYERS_A, D_MODEL, A_IN), D_MODEL ** -0.5)
    b_i = -1.0 + rnd((N_LAYERS_A, A_HEADS), 0.1)
    b_f = jnp.linspace(3.0, 6.0, A_HEADS, dtype=F32)[None, :] + rnd((N_LAYERS_A, A_HEADS), 0.1)
    a_b_gates = jnp.concatenate([b_i, b_f], axis=-1)
    a_g_out = gain((N_LAYERS_A, A_HEADS, A_DV))
    a_w_out = rnd((N_LAYERS_A, A_HEADS * A_DV, D_MODEL), OUT_SCALE * (A_HEADS * A_DV) ** -0.5)
    b_w_in = rnd((N_LAYERS_B, D_MODEL, B_IN), D_MODEL ** -0.5)
    b_g_q = gain((N_LAYERS_B, B_DH))
    b_g_k = gain((N_LAYERS_B, B_DH))
    b_w_out = rnd((N_LAYERS_B, B_HEADS * B_DH, D_MODEL), OUT_SCALE * (B_HEADS * B_DH) ** -0.5)
    c_w_in = rnd((N_LAYERS_C, D_MODEL, C_IN), D_MODEL ** -0.5)
    c_g_out = gain((N_LAYERS_C, C_HEADS, C_DV))
    c_w_out = rnd((N_LAYERS_C, C_HEADS * C_DV, D_MODEL), OUT_SCALE * (C_HEADS * C_DV) ** -0.5)
    d_w_in = rnd((N_LAYERS_D, D_MODEL, 2 * D_RNN), D_MODEL ** -0.5)
    d_conv_w = rnd((N_LAYERS_D, D_CONV, D_RNN), D_CONV ** -0.5)
    d_conv_b = rnd((N_LAYERS_D, D_RNN), 0.02)
    d_w_gates = rnd((N_LAYERS_D, D_BLOCKS, D_BW, 2 * D_BW), D_BW ** -0.5)
    d_b_gates = rnd((N_LAYERS_D, D_BLOCKS, 2 * D_BW), 0.1)
    a0 = jax.random.uniform(ks.pop(), (N_LAYERS_D, D_RNN), F32, minval=0.9, maxval=0.999)
    d_lru = jnp.log(a0) - jnp.log1p(-a0)
    d_w_out = rnd((N_LAYERS_D, D_RNN, D_MODEL), OUT_SCALE * D_RNN ** -0.5)
    return {'x': x, 'positions': positions, 'norm_mix': norm_mix, 'norm_ffn': norm_ffn,
            'ffn_w_in': ffn_w_in, 'ffn_w_out': ffn_w_out,
            'a_w_in': a_w_in, 'a_b_gates': a_b_gates, 'a_g_out': a_g_out, 'a_w_out': a_w_out,
            'b_w_in': b_w_in, 'b_g_q': b_g_q, 'b_g_k': b_g_k, 'b_w_out': b_w_out,
            'c_w_in': c_w_in, 'c_g_out': c_g_out, 'c_w_out': c_w_out,
            'd_w_in': d_w_in, 'd_conv_w': d_conv_w, 'd_conv_b': d_conv_b, 'd_w_gates': d_w_gates,
            'd_b_gates': d_b_gates, 'd_lru': d_lru, 'd_w_out': d_w_out}


def reference(x, positions, norm_mix, norm_ffn, ffn_w_in, ffn_w_out,
              a_w_in, a_b_gates, a_g_out, a_w_out,
              b_w_in, b_g_q, b_g_k, b_w_out,
              c_w_in, c_g_out, c_w_out,
              d_w_in, d_conv_w, d_conv_b, d_w_gates, d_b_gates, d_lru, d_w_out):
    h = x
    for layer in range(DEPTH):
        mixer, r = layer % N_MIXERS, layer // N_MIXERS
        u = rmsnorm(h, norm_mix[layer])
        if mixer == 0:
            y = mlstm_mixer(u, a_w_in[r], a_b_gates[r], a_g_out[r], a_w_out[r])
        elif mixer == 1:
            y = moba_mixer(u, positions, b_w_in[r], b_g_q[r], b_g_k[r], b_w_out[r])
        elif mixer == 2:
            y = retention_mixer(u, positions, c_w_in[r], c_g_out[r], c_w_out[r])
        else:
            y = rglru_mixer(u, d_w_in[r], d_conv_w[r], d_conv_b[r], d_w_gates[r], d_b_gates[r], d_lru[r], d_w_out[r])
        h = h + y
        h = h + swiglu(rmsnorm(h, norm_ffn[layer]), ffn_w_in[layer], ffn_w_out[layer])
    return h
```

```python
import contextlib
import math
import os
import numpy as np
import ml_dtypes
import concourse.bass as bass
import concourse.mybir as mybir
from concourse.bass_utils import run_bass_kernel_spmd

F32 = mybir.dt.float32
BF16 = mybir.dt.bfloat16
I32 = mybir.dt.int32
AF = mybir.ActivationFunctionType
ALU = mybir.AluOpType
AX = mybir.AxisListType

D = 1024
DFF = 2816
EPS = 1e-6
ENGS = ("pe", "act", "dve", "pool", "sp")


class Buf:
    __slots__ = ("name", "w", "r", "rd", "excl")

    def __init__(self, name="", excl=False):
        self.name = name
        self.w = None
        self.r = {}
        self.rd = []
        self.excl = excl


class Slot:
    __slots__ = ("sem", "count", "last")

    def __init__(self, sem):
        self.sem = sem
        self.count = 0
        self.last = None


class Op:
    __slots__ = ("eng", "fn", "deps", "pos", "sig", "sigidx", "waits", "slot", "slotval")

    def __init__(self, eng, fn):
        self.eng = eng
        self.fn = fn
        self.deps = {}
        self.pos = -1
        self.sig = False
        self.sigidx = 0
        self.waits = []
        self.slot = None
        self.slotval = 0


class Prog:
    def __init__(self):
        self.ops = {e: [] for e in ENGS}
        self.all = []
        self.slots = []

    def add(self, eng, fn, reads=(), writes=(), slot=None, extra_deps=()):
        op = Op(eng, fn)
        deps = op.deps
        for b in reads:
            if b.w is not None:
                deps[b.w] = "raw"
            if b.excl:
                for r in b.r.values():
                    if r.eng != eng:
                        deps.setdefault(r, "war")
        for b in writes:
            if b.w is not None:
                deps.setdefault(b.w, "waw")
            for r in b.r.values():
                deps.setdefault(r, "war")
            for r in b.rd:
                deps.setdefault(r, "war")
        for d in extra_deps:
            if d is not None:
                deps[d] = "raw"
        if slot is not None:
            if slot.last is not None:
                deps[slot.last] = "raw"
            slot.count += 1
            op.slot = slot
            op.slotval = 16 * slot.count
            slot.last = op
        deps.pop(op, None)
        for b in writes:
            b.w = op
            b.r = {}
            b.rd = []
        for b in reads:
            if slot is not None:
                b.rd.append(op)
            else:
                b.r[eng] = op
        op.pos = len(self.ops[eng])
        self.ops[eng].append(op)
        self.all.append(op)
        return op

    def barrier(self):
        deps = []
        for e in ENGS:
            for op in reversed(self.ops[e]):
                if op.fn is not None:
                    deps.append(op)
                    break
        for s in self.slots:
            if s.last is not None:
                deps.append(s.last)
        b1 = self.add("sp", lambda e: e.nop(), extra_deps=deps)
        for e in ENGS:
            if e != "sp":
                self.add(e, None, extra_deps=[b1])
        return b1

    def finalize(self, sems):
        known = {x: {y: -1 for y in ENGS} for x in ENGS}
        knownslot = {x: {} for x in ENGS}
        for op in self.all:
            x = op.eng
            for dep, kind in op.deps.items():
                if dep.slot is not None:
                    if knownslot[x].get(dep.slot, 0) >= dep.slotval:
                        continue
                    knownslot[x][dep.slot] = dep.slotval
                    op.waits.append((dep.slot.sem, dep.slotval))
                else:
                    y = dep.eng
                    if y == x and (x == "pe" or kind == "war"):
                        continue
                    if known[x][y] >= dep.pos:
                        continue
                    known[x][y] = dep.pos
                    dep.sig = True
                    op.waits.append((y, dep))
        for e in ENGS:
            c = 0
            for op in self.ops[e]:
                if op.sig:
                    c += 1
                    op.sigidx = c
        self.sems = sems

    def emit_engine(self, ename, eng):
        sems = self.sems
        for op in self.ops[ename]:
            for w in op.waits:
                if isinstance(w[0], str):
                    eng.wait_ge(sems[w[0]], w[1].sigidx)
                else:
                    eng.wait_ge(w[0], w[1])
            if op.fn is None:
                assert not op.sig
                continue
            inst = op.fn(eng)
            if op.slot is not None:
                inst.then_inc(op.slot.sem, 16)
            elif op.sig:
                inst.then_inc(sems[ename], 1)

    def emit(self, block):
        block.tensor(lambda e: self.emit_engine("pe", e))
        block.scalar(lambda e: self.emit_engine("act", e))
        block.vector(lambda e: self.emit_engine("dve", e))
        block.gpsimd(lambda e: self.emit_engine("pool", e))
        block.sync(lambda e: self.emit_engine("sp", e))


PARAM_SHAPES = {
    "norm_mix": [4, 1024], "norm_ffn": [4, 1024],
    "ffn_w_in": [4, 1024, 5632], "ffn_w_out": [4, 2816, 1024],
    "a_w_in": [1, 1024, 3080], "a_b_gates": [1, 8], "a_g_out": [1, 4, 256], "a_w_out": [1, 1024, 1024],
    "b_w_in": [1, 1024, 3072], "b_g_q": [1, 128], "b_g_k": [1, 128], "b_w_out": [1, 1024, 1024],
    "c_w_in": [1, 1024, 6144], "c_g_out": [1, 4, 512], "c_w_out": [1, 2048, 1024],
    "d_w_in": [1, 1024, 2048], "d_conv_w": [1, 4, 1024], "d_conv_b": [1, 1024],
    "d_w_gates": [1, 4, 256, 512], "d_b_gates": [1, 4, 512], "d_lru": [1, 1024], "d_w_out": [1, 1024, 1024],
}
DEV_PARAMS = [k for k in PARAM_SHAPES if k not in ("d_conv_w", "d_conv_b", "d_b_gates", "d_lru")]


def pack_d_small(inputs):
    cw = np.asarray(inputs["d_conv_w"], np.float32)[0].reshape(4, 8, 128).transpose(2, 0, 1).reshape(128, 32)
    cb = np.asarray(inputs["d_conv_b"], np.float32)[0].reshape(8, 128).T
    bg = np.asarray(inputs["d_b_gates"], np.float32)[0].reshape(4, 4, 128).transpose(2, 0, 1).reshape(128, 16)
    lr = np.asarray(inputs["d_lru"], np.float32)[0].reshape(8, 128).T
    return np.ascontiguousarray(np.concatenate([cw, cb, bg, lr], axis=1))


def host_consts():
    c = {}
    c["k_ident"] = np.eye(128, dtype=np.float32).astype(ml_dtypes.bfloat16)
    tri = (np.arange(128)[:, None] <= np.arange(128)[None, :]).astype(np.float32)
    c["k_tri"] = tri
    c["k_tribf"] = tri.astype(ml_dtypes.bfloat16)
    c["k_ones"] = np.ones((128, 128), np.float32)
    invb = (500000.0 ** (-np.arange(16, dtype=np.float32) * (2.0 / 32))).astype(np.float32)
    c["k_invb"] = np.broadcast_to(invb[None, :], (128, 16)).copy()
    invc = (10000.0 ** (-np.arange(128, dtype=np.float32) * (2.0 / 256))).astype(np.float32)
    c["k_invc"] = np.broadcast_to(invc[None, :], (128, 128)).copy()
    ret = np.zeros((128, 8), np.float64)
    p = np.arange(128, dtype=np.float64)
    for hh in range(4):
        lg = np.log1p(-np.exp2(-5.0 - hh))
        ret[:, hh] = np.exp((p + 1.0) * lg)
        ret[:, 4 + hh] = np.exp(-(p + 1.0) * lg) * (256 ** -0.5)
    c["k_ret"] = ret.astype(np.float32)
    return c


CONST_SPECS = {"k_ident": ([128, 128], BF16), "k_tri": ([128, 128], F32), "k_tribf": ([128, 128], BF16),
               "k_ones": ([128, 128], F32), "k_invb": ([128, 16], F32), "k_invc": ([128, 128], F32),
               "k_ret": ([128, 8], F32)}
RET_GCHUNK = [float(np.exp(128.0 * np.log1p(-np.exp2(-5.0 - hh)))) for hh in range(4)]


def build(nseq=2, S=2048, plan=None):
    if plan is None:
        plan = [("mix", l) if k == 0 else ("ffn", l) for l in range(4) for k in range(2)]
    NT = S // 128
    NG = S // 512
    nc = bass.Bass("TRN2", target_bir_lowering=False)
    x_d = nc.dram_tensor("x", [nseq, S, D], F32, kind="ExternalInput").ap()
    pos_d = nc.dram_tensor("positions", [nseq, 128, S // 128], I32, kind="ExternalInput").ap()
    W = {k: nc.dram_tensor(k, PARAM_SHAPES[k], F32, kind="ExternalInput").ap() for k in DEV_PARAMS}
    W["d_small"] = nc.dram_tensor("d_small", [128, 64], F32, kind="ExternalInput").ap()
    KC = {k: nc.dram_tensor(k, v[0], v[1], kind="ExternalInput").ap() for k, v in CONST_SPECS.items()}
    out_d = nc.dram_tensor("out", [nseq, S, D], F32, kind="ExternalOutput").ap()

    P = Prog()
    with contextlib.ExitStack() as st:
        def sb(name, shape, dt):
            return st.enter_context(nc.sbuf_tensor(name, shape, dt))

        def pst(name, shape, dt):
            return st.enter_context(nc.psum_tensor(name, shape, dt))

        def newsem(name):
            return st.enter_context(nc.semaphore(name))

        sems = {e: newsem("s_" + e) for e in ENGS}
        nslots = [0]

        def newslot():
            s = Slot(newsem("d%d" % nslots[0]))
            nslots[0] += 1
            P.slots.append(s)
            return s

        h = sb("h", [128, NT, D], F32)
        Bh = [Buf("h%d" % t) for t in range(NT)]
        uT = sb("uT", [128, 8, S], BF16)
        BuT = [Buf("uT%d" % t) for t in range(NT)]
        NWS = 4
        wslot = [sb("ws%d" % i, [128, 4096], BF16) for i in range(NWS)]
        Bws = [Buf("ws%d" % i) for i in range(NWS)]
        ws_dslots = [(newslot(), newslot()) for _ in range(NWS)]
        ws_rr = [0]
        gbc = sb("gbc", [128, D], F32)
        Bgbc = Buf("gbc")
        gbc_slot = newslot()
        ARENA = 16640
        arena = sb("arena", [128, ARENA], F32)
        arena_bf = arena[:].bitcast(BF16)
        scr = sb("scr", [128, D], BF16)
        Bscr = Buf("scr")
        ubf = [sb("ubf%d" % i, [128, D], BF16) for i in range(2)]
        Bubf = [Buf("ubf%d" % i) for i in range(2)]
        stat = sb("stat", [128, 3 * NT], F32)
        Bstat = [Buf("stat%d" % t) for t in range(NT)]
        ident = sb("ident", [128, 128], BF16)
        tri = sb("tri", [128, 128], F32)
        tribf = sb("tribf", [128, 128], BF16)
        ones = sb("ones", [128, 128], F32)
        invb = sb("invb", [128, 16], F32)
        invc = sb("invc", [128, 128], F32)
        retc = sb("retc", [128, 8], F32)
        neghalf = sb("neghalf", [128, 2], F32)
        Bconst = Buf("const")
        posi = sb("posi", [128, NT], I32)
        posf = sb("posf", [128, NT], F32)
        Bpos = Buf("pos")

        NPS = 6
        psf = [pst("psf%d" % i, [128, 512], F32) for i in range(NPS)]
        Bpsf = [Buf("psf%d" % i, excl=True) for i in range(NPS)]
        psb = [pst("psb%d" % i, [128, 1024], BF16) for i in range(2)]
        Bpsb = [Buf("psb%d" % i, excl=True) for i in range(2)]
        rr = {"psf": 0, "psb": 0, "io": 0, "ubf": 0}

        def next_psf():
            i = rr["psf"]
            rr["psf"] = (i + 1) % NPS
            return psf[i], Bpsf[i]

        def next_psb():
            i = rr["psb"]
            rr["psb"] = (i + 1) % 2
            return psb[i], Bpsb[i]

        io_slots = [newslot() for _ in range(4)]

        def next_io():
            i = rr["io"]
            rr["io"] = (i + 1) % 4
            return io_slots[i]

        def next_ws():
            i = ws_rr[0]
            ws_rr[0] = (i + 1) % NWS
            return i

        def dma(eng, out, in_, reads, writes, slot):
            return P.add(eng, lambda e: e.dma_start(out=out, in_=in_), reads=reads, writes=writes, slot=slot)

        def wload(i, part, out, in_):
            return dma("pool", out, in_, [], [Bws[i]], ws_dslots[i][part])

        def mm(out, lhsT, rhs, start, stop, reads, writes):
            return P.add("pe", lambda e: e.matmul(out, lhsT=lhsT, rhs=rhs, start=start, stop=stop),
                         reads=reads, writes=writes)

        def tr(out, in_, reads, writes):
            return P.add("pe", lambda e: e.transpose(out=out, in_=in_, identity=ident[:]),
                         reads=list(reads) + [Bconst], writes=writes)

        def act(out, in_, func, reads, writes, **kw):
            return P.add("act", lambda e: e.activation(out=out, in_=in_, func=func, **kw), reads=reads, writes=writes)

        def vtt(out, in0, in1, op, reads, writes, eng="dve"):
            return P.add(eng, lambda e: e.tensor_tensor(out=out, in0=in0, in1=in1, op=op), reads=reads, writes=writes)

        def vts(out, in0, s1, s2, op0, op1, reads, writes, eng="dve"):
            if op1 is None:
                return P.add(eng, lambda e: e.tensor_scalar(out=out, in0=in0, scalar1=s1, scalar2=None, op0=op0),
                             reads=reads, writes=writes)
            return P.add(eng, lambda e: e.tensor_scalar(out=out, in0=in0, scalar1=s1, scalar2=s2, op0=op0, op1=op1),
                         reads=reads, writes=writes)

        def vstt(out, in0, scalar, in1, op0, op1, reads, writes):
            return P.add("dve", lambda e: e.scalar_tensor_tensor(out=out, in0=in0, scalar=scalar, in1=in1,
                                                                 op0=op0, op1=op1), reads=reads, writes=writes)

        def vcopy(out, in_, reads, writes, eng="dve"):
            return P.add(eng, lambda e: e.tensor_copy(out=out, in_=in_), reads=reads, writes=writes)

        def vrecip(out, in_, reads, writes):
            return P.add("dve", lambda e: e.reciprocal(out=out, in_=in_), reads=reads, writes=writes)

        def memset(ap, val, writes, eng="dve"):
            return P.add(eng, lambda e: e.memset(ap, val), writes=writes)

        for name, t in (("k_ident", ident), ("k_tri", tri), ("k_tribf", tribf), ("k_ones", ones),
                        ("k_invb", invb), ("k_invc", invc), ("k_ret", retc)):
            dma("sp", t[:], KC[name], [], [Bconst], next_io())

        memset(neghalf[:], -0.5, [Bconst], eng="pool")

        def norm_phase(g_row):
            dma("sp", gbc[:], g_row.partition_broadcast(128), [], [Bgbc], gbc_slot)
            for t in range(NT):
                ss = stat[:, 3 * t:3 * t + 1]
                sq = stat[:, 3 * t + 1:3 * t + 2]
                rs = stat[:, 3 * t + 2:3 * t + 3]
                act(scr[:], h[:, t, :], AF.Square, [Bh[t]], [Bscr, Bstat[t]], accum_out=ss)
                act(sq, ss, AF.Sqrt, [Bstat[t]], [Bstat[t]], scale=1.0 / D, bias=EPS)
                vrecip(rs, sq, [Bstat[t]], [Bstat[t]])
                ui = rr["ubf"]
                rr["ubf"] = 1 - ui
                vstt(ubf[ui][:], h[:, t, :], rs, gbc[:], ALU.mult, ALU.mult, [Bh[t], Bstat[t], Bgbc], [Bubf[ui]])
                pb, Bpb = next_psb()
                for k in range(8):
                    tr(pb[:, k * 128:(k + 1) * 128], ubf[ui][:, k * 128:(k + 1) * 128], [Bubf[ui]], [Bpb])
                vcopy(uT[:, :, t * 128:(t + 1) * 128], pb[:].rearrange("p (k c) -> p k c", k=8), [Bpb], [BuT[t]])

        def ffn_phase(l):
            w_in = W["ffn_w_in"][l].rearrange("(kc p) n -> p kc n", p=128)
            w_out = W["ffn_w_out"][l]
            hT = arena_bf[:, 0:6 * S].rearrange("p (c s) -> p c s", c=6)
            BhT = [[Buf() for _ in range(NG)] for _ in range(6)]
            wo = [arena_bf[:, 6 * S + i * 6144: 6 * S + (i + 1) * 6144].rearrange("p (c n) -> p c n", c=6)
                  for i in range(2)]
            Bwo = [Buf(), Buf()]
            wo_slot = [newslot(), newslot()]
            sg = [arena[:, ARENA - 1024 + i * 512: ARENA - 1024 + (i + 1) * 512] for i in range(2)]
            assert 6 * S + 2 * 6144 <= 2 * ARENA - 2048
            Bsg = [Buf(), Buf()]
            sgi = 0
            quarters = [[0, 1, 2], [3, 4, 5], [6, 7, 8], [9, 10]]
            for qi, groups in enumerate(quarters):
                nch = 2 * len(groups)
                r0 = groups[0] * 256
                wi = qi % 2
                dma("pool", wo[wi][:, 0:nch, :], w_out[r0:r0 + nch * 128, :].rearrange("(c p) n -> p c n", p=128),
                    [], [Bwo[wi]], wo_slot[wi])
                for gi, g in enumerate(groups):
                    si = next_ws()
                    wsv = wslot[si][:].rearrange("p (k n) -> p k n", k=8)
                    wload(si, 0, wsv[:, :, 0:256], w_in[:, :, g * 256:(g + 1) * 256])
                    wload(si, 1, wsv[:, :, 256:512], w_in[:, :, DFF + g * 256:DFF + (g + 1) * 256])
                    for half in range(2):
                        ch = gi * 2 + half
                        for tg in range(NG):
                            pg, Bpg = next_psf()
                            pu, Bpu = next_psf()
                            ur = [BuT[tg * 4 + i] for i in range(4)]
                            for kc in range(8):
                                mm(pg[:], wsv[:, kc, half * 128:(half + 1) * 128], uT[:, kc, tg * 512:(tg + 1) * 512],
                                   kc == 0, kc == 7, [Bws[si]] + ur, [Bpg])
                            for kc in range(8):
                                mm(pu[:], wsv[:, kc, 256 + half * 128:256 + (half + 1) * 128],
                                   uT[:, kc, tg * 512:(tg + 1) * 512], kc == 0, kc == 7, [Bws[si]] + ur, [Bpu])
                            act(sg[sgi][:], pg[:], AF.Silu, [Bpg], [Bsg[sgi]])
                            vtt(hT[:, ch, tg * 512:(tg + 1) * 512], pu[:], sg[sgi][:], ALU.mult,
                                [Bpu, Bsg[sgi]], [BhT[ch][tg]])
                            sgi = 1 - sgi
                for t in range(NT):
                    for nh in range(2):
                        po, Bpo = next_psf()
                        for c in range(nch):
                            mm(po[:], hT[:, c, t * 128:(t + 1) * 128], wo[wi][:, c, nh * 512:(nh + 1) * 512],
                               c == 0, c == nch - 1, [BhT[c][t // 4], Bwo[wi]], [Bpo])
                        vtt(h[:, t, nh * 512:(nh + 1) * 512], po[:], h[:, t, nh * 512:(nh + 1) * 512], ALU.add,
                            [Bpo, Bh[t]], [Bh[t]])

        class Arena:
            def __init__(self):
                self.off = 0

            def f32(self, n):
                o = self.off
                self.off += n
                assert self.off <= ARENA, self.off
                return arena[:, o:o + n]

            def bf(self, n):
                o = self.off
                self.off += (n + 1) // 2
                assert self.off <= ARENA, self.off
                return arena_bf[:, 2 * o:2 * o + n]

            def i32(self, n):
                return self.f32(n).bitcast(I32)

        def mkset(A, spec):
            d = {}
            for name, kind, n in spec:
                d[name] = A.f32(n) if kind == "f" else A.bf(n)
                d["B" + name] = Buf()
            return d

        def bc_mid(ap2d, n):
            return ap2d.unsqueeze(1).broadcast_to([ap2d.shape[0], n, ap2d.shape[1]])

        def proj_tm(t, wsv, c0, n, Bw):
            ps, Bp = next_psf()
            for kc in range(8):
                mm(ps[:, 0:n], uT[:, kc, t * 128:(t + 1) * 128], wsv[:, kc, c0:c0 + n], kc == 0, kc == 7,
                   [BuT[t], Bw], [Bp])
            return ps, Bp

        def proj_fm(tg, wsv, c0, Bw):
            ps, Bp = next_psf()
            ur = [BuT[tg * 4 + i] for i in range(4)]
            for kc in range(8):
                mm(ps[:], wsv[:, kc, c0:c0 + 128], uT[:, kc, tg * 512:(tg + 1) * 512], kc == 0, kc == 7,
                   [Bw] + ur, [Bp])
            return ps, Bp

        def outproj_acc(t, lhs_list, wfn, Bw):
            n = len(lhs_list)
            for nh in range(2):
                po, Bpo = next_psf()
                for c, (lap, lb) in enumerate(lhs_list):
                    mm(po[:], lap, wfn(c, nh), c == 0, c == n - 1, [lb, Bw], [Bpo])
                vtt(h[:, t, nh * 512:(nh + 1) * 512], po[:], h[:, t, nh * 512:(nh + 1) * 512], ALU.add,
                    [Bpo, Bh[t]], [Bh[t]])

        def trig_tables(A, inv_tile, ni, cos_out, sin_out, Bt):
            n = NT * ni
            ang = A.f32(n)
            angs = A.f32(n)
            kf = A.f32(n)
            ki = A.i32(n)
            Bl = Buf()
            ang3 = ang.rearrange("p (t i) -> p t i", i=ni)
            for t in range(NT):
                vts(ang3[:, t, :], inv_tile, posf[:, t:t + 1], None, ALU.mult, None, [Bconst, Bpos], [Bl])
            PI = math.pi
            for shift, out in ((0.0, sin_out), (PI / 2, cos_out)):
                vts(angs, ang, shift, None, ALU.add, None, [Bl], [Bl])
                vts(kf, angs, 1.0 / (2 * PI), None, ALU.mult, None, [Bl], [Bl])
                vcopy(ki, kf, [Bl], [Bl])
                vcopy(kf, ki, [Bl], [Bl])
                vstt(angs, kf, -2 * PI, angs, ALU.mult, ALU.add, [Bl], [Bl])
                vts(angs, angs, -3.14159, 3.14159, ALU.max, ALU.min, [Bl], [Bl])
                act(out, angs, AF.Sin, [Bl], [Bt, Bl])

        def pipeline(n, stages):
            ns = len(stages)
            for k in range(n + ns - 1):
                for s_, f in enumerate(stages):
                    i = k - s_
                    if 0 <= i < n:
                        f(i)

        def rglru_phase(l):
            A = Arena()
            w_in = W["d_w_in"][0].rearrange("(kc p) n -> p kc n", p=128)
            small = A.f32(64)
            Bsm = Buf()
            dma("sp", small, W["d_small"], [], [Bsm], next_io())
            cw = small[:, 0:32]
            cb = small[:, 32:40]
            bg = small[:, 40:56]
            lru = small[:, 56:64]
            cvh = A.f32(8)
            cv1 = A.f32(8)
            bgh = A.f32(16)
            tmp8 = A.f32(8)
            act(tmp8, lru, AF.Exp, [Bsm], [Bsm], scale=-1.0)
            act(tmp8, tmp8, AF.Ln, [Bsm], [Bsm], bias=1.0)
            vts(cvh, tmp8, -4.0, None, ALU.mult, None, [Bsm], [Bsm])
            vts(cv1, tmp8, -8.0, None, ALU.mult, None, [Bsm], [Bsm])
            vts(bgh, bg, 0.5, None, ALU.mult, None, [Bsm], [Bsm])
            sets = [mkset(A, [("xbp0", "f", 515), ("xbp1", "f", 515), ("gb0", "f", 512), ("gb1", "f", 512),
                              ("xc0", "f", 512), ("xc1", "f", 512), ("xcb0", "b", 512), ("xcb1", "b", 512),
                              ("ri0", "f", 512), ("ri1", "f", 512), ("ri2", "f", 512), ("ri3", "f", 512),
                              ("yT0", "b", 512), ("yT1", "b", 512)]) for _ in range(2)]
            chs = [mkset(A, [("av", "f", 512), ("a2", "f", 512), ("bt", "f", 512), ("hs", "f", 512), ("g2", "f", 512)])] * 2
            hcar = A.f32(2)
            Bhc = Buf()
            wctx = {}

            def load_w(n):
                si = next_ws()
                wsv = wslot[si][:].rearrange("p (k n) -> p k n", k=8)
                wload(si, 0, wsv[:, :, 0:256], w_in[:, :, n * 256:(n + 1) * 256])
                wload(si, 1, wsv[:, :, 256:512], w_in[:, :, 1024 + n * 256:1024 + (n + 1) * 256])
                s2 = next_ws()
                wg = wslot[s2][:, 0:1024].rearrange("p (c e) -> p c e", c=2)
                wo = wslot[s2][:, 1024:3072].rearrange("p (c e) -> p c e", c=2)
                wload(s2, 0, wg, W["d_w_gates"][0, n].rearrange("(c p) e -> p c e", p=128))
                wload(s2, 1, wo, W["d_w_out"][0, n * 256:(n + 1) * 256, :].rearrange("(c p) e -> p c e", p=128))
                wctx[n] = (si, wsv, s2, wg, wo)

            def st0(it):
                n, tg = divmod(it, NG)
                if tg == 0:
                    load_w(n)
                si, wsv, s2, wg, wo = wctx[n]
                T = sets[it % 2]
                Tp = sets[(it + 1) % 2]
                for ch in range(2):
                    cc = n * 2 + ch
                    xbp, Bxbp = T["xbp%d" % ch], T["Bxbp%d" % ch]
                    gb, Bgb = T["gb%d" % ch], T["Bgb%d" % ch]
                    xc, Bxc = T["xc%d" % ch], T["Bxc%d" % ch]
                    xcb, Bxcb = T["xcb%d" % ch], T["Bxcb%d" % ch]
                    if tg == 0:
                        memset(xbp[:, 0:3], 0.0, [Bxbp])
                    else:
                        vcopy(xbp[:, 0:3], Tp["xbp%d" % ch][:, 512:515], [Tp["Bxbp%d" % ch]], [Bxbp])
                    px, Bpx = proj_fm(tg, wsv, 256 + ch * 128, Bws[si])
                    act(xbp[:, 3:515], px[:], AF.Copy, [Bpx], [Bxbp])
                    pg, Bpg = proj_fm(tg, wsv, ch * 128, Bws[si])
                    act(gb, pg[:], AF.Copy, [Bpg], [Bgb])
                    vts(xc, xbp[:, 0:512], cw[:, cc:cc + 1], cb[:, cc:cc + 1], ALU.mult, ALU.add,
                        [Bxbp, Bsm], [Bxc])
                    for j in range(1, 4):
                        vstt(xc, xbp[:, j:j + 512], cw[:, j * 8 + cc:j * 8 + cc + 1], xc,
                             ALU.mult, ALU.add, [Bxbp, Bsm, Bxc], [Bxc])
                    act(xcb, xc, AF.Copy, [Bxc], [Bxcb])

            def st1(it):
                n, tg = divmod(it, NG)
                si, wsv, s2, wg, wo = wctx[n]
                T = sets[it % 2]
                for ec in range(4):
                    pgt, Bpgt = next_psf()
                    for c2 in range(2):
                        mm(pgt[:], wg[:, c2, ec * 128:(ec + 1) * 128], T["xcb%d" % c2], c2 == 0, c2 == 1,
                           [Bws[s2], T["Bxcb%d" % c2]], [Bpgt])
                    act(T["ri%d" % ec], pgt[:], AF.Tanh, [Bpgt, Bsm], [T["Bri%d" % ec]],
                        bias=bgh[:, n * 4 + ec:n * 4 + ec + 1], scale=0.5)
                for ch in range(2):
                    cc = n * 2 + ch
                    C = chs[ch]
                    av, a2, bt, hs, g2 = C["av"], C["a2"], C["bt"], C["hs"], C["g2"]
                    Bav, Ba2, Bbt, Bhs, Bg2 = C["Bav"], C["Ba2"], C["Bbt"], C["Bhs"], C["Bg2"]
                    tr_, Btr = T["ri%d" % ch], T["Bri%d" % ch]
                    ti_, Bti = T["ri%d" % (2 + ch)], T["Bri%d" % (2 + ch)]
                    gb, Bgb = T["gb%d" % ch], T["Bgb%d" % ch]
                    xc, Bxc = T["xc%d" % ch], T["Bxc%d" % ch]
                    yT, ByT = T["yT%d" % ch], T["ByT%d" % ch]
                    act(av, tr_, AF.Exp, [Btr, Bsm], [Bav], scale=cvh[:, cc:cc + 1], bias=cvh[:, cc:cc + 1])
                    act(a2, tr_, AF.Exp, [Btr, Bsm], [Ba2], scale=cv1[:, cc:cc + 1], bias=cv1[:, cc:cc + 1])
                    act(a2, a2, AF.Sqrt, [Ba2], [Ba2], scale=-1.0, bias=1.0)
                    vstt(bt, ti_, 1.0, xc, ALU.add, ALU.mult, [Bti, Bxc], [Bbt])
                    vstt(bt, bt, 0.5, a2, ALU.mult, ALU.mult, [Bbt, Ba2], [Bbt])
                    init = 0.0 if tg == 0 else hcar[:, ch:ch + 1]
                    P.add("dve", lambda e, init=init, hs=hs, av=av, bt=bt: e.tensor_tensor_scan(
                        out=hs, data0=av, data1=bt, initial=init, op0=ALU.mult, op1=ALU.add),
                          reads=[Bav, Bbt, Bhc], writes=[Bhs])
                    vcopy(hcar[:, ch:ch + 1], hs[:, 511:512], [Bhs], [Bhc])
                    act(g2, gb, AF.Square, [Bgb], [Bg2])
                    vts(g2, g2, 0.044715, 1.0, ALU.mult, ALU.add, [Bg2], [Bg2])
                    vtt(g2, g2, gb, ALU.mult, [Bg2, Bgb], [Bg2])
                    act(g2, g2, AF.Tanh, [Bg2], [Bg2], scale=0.7978845608028654)
                    vstt(g2, g2, 1.0, gb, ALU.add, ALU.mult, [Bg2, Bgb], [Bg2])
                    vstt(yT, hs, 0.5, g2, ALU.mult, ALU.mult, [Bhs, Bg2], [ByT])
                for j in range(4):
                    t = tg * 4 + j
                    outproj_acc(t, [(T["yT%d" % c][:, j * 128:(j + 1) * 128], T["ByT%d" % c]) for c in range(2)],
                                lambda c, nh: wo[:, c, nh * 512:(nh + 1) * 512], Bws[s2])

            pipeline(4 * NG, [st0, st1])

        def mlstm_phase(l):
            A = Arena()
            w_in = W["a_w_in"][0].rearrange("(kc p) n -> p kc n", p=128)
            bgt = A.f32(8)
            gob = A.f32(1024)
            Bsm = Buf()
            dma("sp", bgt, W["a_b_gates"][0].partition_broadcast(128), [], [Bsm], next_io())
            dma("sp", gob, W["a_g_out"][0].rearrange("h v -> (h v)").partition_broadcast(128), [], [Bsm], next_io())
            gl = A.f32(NT * 8)
            gl3 = gl.rearrange("p (t g) -> p t g", g=8)
            Bgl = Buf()
            si = next_ws()
            wsv = wslot[si][:].rearrange("p (k n) -> p k n", k=8)
            wload(si, 0, wsv[:, :, 0:8], w_in[:, :, 3072:3080])
            for t in range(NT):
                ps, Bp = proj_tm(t, wsv, 0, 8, Bws[si])
                vtt(gl3[:, t, :], ps[:, 0:8], bgt, ALU.add, [Bp, Bsm], [Bgl])
            n4 = NT * 4
            xf = gl3[:, :, 4:8]
            xi = gl3[:, :, 0:4]
            t1 = A.f32(n4)
            t13 = t1.rearrange("p (t g) -> p t g", g=4)
            lf = A.f32(n4)
            lf3 = lf.rearrange("p (t g) -> p t g", g=4)
            bcs = A.f32(n4)
            bcs3 = bcs.rearrange("p (t g) -> p t g", g=4)
            ea = A.f32(n4)
            ea3 = ea.rearrange("p (t g) -> p t g", g=4)
            eb = A.f32(n4)
            eb3 = eb.rearrange("p (t g) -> p t g", g=4)
            ebt = A.f32(n4)
            ebt3 = ebt.rearrange("p (t g) -> p t g", g=4)
            act(t13, xf, AF.Abs, [Bgl], [Bgl])
            act(t1, t1, AF.Exp, [Bgl], [Bgl], scale=-1.0)
            act(t1, t1, AF.Ln, [Bgl], [Bgl], bias=1.0)
            vts(lf3, xf, 0.0, None, ALU.min, None, [Bgl], [Bgl])
            vtt(lf, lf, t1, ALU.subtract, [Bgl], [Bgl])
            pc, Bpc = next_psf()
            mm(pc[:, 0:n4], tri[:], lf, True, True, [Bconst, Bgl], [Bpc])
            vcopy(bcs, pc[:, 0:n4], [Bpc], [Bgl])
            pt, Bpt = next_psf()
            mm(pt[:, 0:n4], ones[:], lf, True, True, [Bconst, Bgl], [Bpt])
            act(ebt, pt[:, 0:n4], AF.Exp, [Bpt], [Bgl])
            act(eb, bcs, AF.Exp, [Bgl], [Bgl])
            vtt(ea3, xi, bcs3, ALU.subtract, [Bgl], [Bgl])
            act(ea, ea, AF.Exp, [Bgl], [Bgl])
            Cst = A.f32(257)
            Cbf = A.bf(258)
            BC, BCb = Buf(), Buf()
            NSET = 3
            sets = [mkset(A, [("qk", "b", 256), ("vaug", "b", 258), ("qkT", "b", 256), ("PT", "b", 128),
                              ("tmpC", "f", 257), ("sm", "f", 8), ("ht", "f", 256), ("og", "f", 256),
                              ("ybf", "b", 256), ("yT", "b", 256)]) for _ in range(NSET)]
            sc = 128.0 ** -0.25
            wctx = {}

            def load_w(hh):
                sa = next_ws()
                wa = wslot[sa][:].rearrange("p (k n) -> p k n", k=8)
                wload(sa, 0, wa[:, :, 0:128], w_in[:, :, hh * 128:(hh + 1) * 128])
                wload(sa, 0, wa[:, :, 128:256], w_in[:, :, 512 + hh * 128:512 + (hh + 1) * 128])
                wload(sa, 1, wa[:, :, 256:512], w_in[:, :, 1024 + hh * 256:1024 + (hh + 1) * 256])
                sb_ = next_ws()
                wb = wslot[sb_][:, 0:2048].rearrange("p (k n) -> p k n", k=8)
                wo = wslot[sb_][:, 2048:4096].rearrange("p (c e) -> p c e", c=2)
                wload(sb_, 0, wb, w_in[:, :, 2048 + hh * 256:2048 + (hh + 1) * 256])
                wload(sb_, 1, wo, W["a_w_out"][0, hh * 256:(hh + 1) * 256, :].rearrange("(c p) e -> p c e", p=128))
                wctx[hh] = (sa, wa, sb_, wb, wo)

            def st0(it):
                hh, c = divmod(it, NT)
                if c == 0:
                    load_w(hh)
                sa, wa, sb_, wb, wo = wctx[hh]
                T = sets[it % NSET]
                p1, Bp1 = proj_tm(c, wa, 0, 512, Bws[sa])
                p2, Bp2 = proj_tm(c, wb, 0, 256, Bws[sb_])
                act(T["qk"], p1[:, 0:256], AF.Copy, [Bp1], [T["Bqk"]], scale=sc)
                act(T["vaug"][:, 0:256], p1[:, 256:512], AF.Copy, [Bp1, Bgl], [T["Bvaug"]], scale=ea3[:, c, hh:hh + 1])
                vcopy(T["vaug"][:, 256:257], ea3[:, c, hh:hh + 1], [Bgl], [T["Bvaug"]])
                act(T["og"], p2[:, 0:256], AF.Sigmoid, [Bp2], [T["Bog"]])
                vtt(T["og"], T["og"], gob[:, hh * 256:(hh + 1) * 256], ALU.mult, [T["Bog"], Bsm], [T["Bog"]])

            def st1(it):
                hh, c = divmod(it, NT)
                sa, wa, sb_, wb, wo = wctx[hh]
                T = sets[it % NSET]
                qk, vaug, qkT, PT, tmpC, sm, ht, og, ybf, yT = [T[k] for k in
                    ("qk", "vaug", "qkT", "PT", "tmpC", "sm", "ht", "og", "ybf", "yT")]
                Bqk, Bva, BqkT, BPT, BtC, Bs, Bht, Bog, Bybf, ByT = [T["B" + k] for k in
                    ("qk", "vaug", "qkT", "PT", "tmpC", "sm", "ht", "og", "ybf", "yT")]
                if c == 0:
                    memset(Cst, 0.0, [BC])
                    memset(Cbf, 0.0, [BCb])
                pb, Bpb = next_psb()
                tr(pb[:, 0:128], qk[:, 0:128], [Bqk], [Bpb])
                tr(pb[:, 128:256], qk[:, 128:256], [Bqk], [Bpb])
                vcopy(qkT, pb[:, 0:256], [Bpb], [BqkT])
                pS, BpS = next_psf()
                mm(pS[:, 0:128], qkT[:, 128:256], qkT[:, 0:128], True, True, [BqkT], [BpS])
                vtt(PT, pS[:, 0:128], tri[:], ALU.mult, [BpS, Bconst], [BPT])
                po, Bpo = next_psf()
                mm(po[:, 0:257], PT, vaug[:, 0:257], True, False, [BPT, Bva], [Bpo])
                mm(po[:, 0:257], qkT[:, 0:128], Cbf[:, 0:257], False, True, [BqkT, BCb], [Bpo])
                pC, BpC = next_psf()
                mm(pC[:, 0:257], qk[:, 128:256], vaug[:, 0:257], True, True, [Bqk, Bva], [BpC])
                vtt(tmpC, pC[:, 0:257], Cst, ALU.add, [BpC, BC], [BtC])
                act(Cst, tmpC, AF.Copy, [BtC, Bgl], [BC], scale=ebt3[:, c, hh:hh + 1])
                act(Cbf[:, 0:257], tmpC, AF.Copy, [BtC, Bgl], [BCb], scale=ebt3[:, c, hh:hh + 1])
                ebc = eb3[:, c, hh:hh + 1]
                act(sm[:, 0:1], po[:, 256:257], AF.Abs, [Bpo, Bgl], [Bs], scale=ebc)
                vts(sm[:, 0:1], sm[:, 0:1], 1.0, None, ALU.max, None, [Bs], [Bs])
                vrecip(sm[:, 1:2], sm[:, 0:1], [Bs], [Bs])
                vtt(sm[:, 2:3], sm[:, 1:2], ebc, ALU.mult, [Bs, Bgl], [Bs])
                act(ht, po[:, 0:256], AF.Copy, [Bpo, Bs], [Bht], scale=sm[:, 2:3])
                act(scr[:, 0:256], ht, AF.Square, [Bht], [Bscr, Bs], accum_out=sm[:, 3:4])

            def st2(it):
                hh, c = divmod(it, NT)
                sa, wa, sb_, wb, wo = wctx[hh]
                T = sets[it % NSET]
                sm, ht, og, ybf, yT = [T[k] for k in ("sm", "ht", "og", "ybf", "yT")]
                Bs, Bht, Bog, Bybf, ByT = [T["B" + k] for k in ("sm", "ht", "og", "ybf", "yT")]
                act(sm[:, 4:5], sm[:, 3:4], AF.Sqrt, [Bs], [Bs], scale=1.0 / 256, bias=EPS)
                vrecip(sm[:, 5:6], sm[:, 4:5], [Bs], [Bs])
                vstt(ybf, ht, sm[:, 5:6], og, ALU.mult, ALU.mult, [Bht, Bs, Bog], [Bybf])
                pb2, Bpb2 = next_psb()
                tr(pb2[:, 0:128], ybf[:, 0:128], [Bybf], [Bpb2])
                tr(pb2[:, 128:256], ybf[:, 128:256], [Bybf], [Bpb2])
                vcopy(yT, pb2[:, 0:256], [Bpb2], [ByT])
                outproj_acc(c, [(yT[:, 0:128], ByT), (yT[:, 128:256], ByT)],
                            lambda cc, nh: wo[:, cc, nh * 512:(nh + 1) * 512], Bws[sb_])

            pipeline(4 * NT, [st0, st1, st2])

        def retention_phase(l):
            A = Arena()
            w_in = W["c_w_in"][0].rearrange("(kc p) n -> p kc n", p=128)
            cos_t = A.f32(NT * 128)
            sin_t = A.f32(NT * 128)
            Btab = Buf()
            mark = A.off
            trig_tables(A, invc[:], 128, cos_t, sin_t, Btab)
            A.off = mark
            cos3 = cos_t.rearrange("p (t i) -> p t i", i=128)
            sin3 = sin_t.rearrange("p (t i) -> p t i", i=128)
            P.barrier()
            gobs = [A.f32(512) for _ in range(2)]
            Bgos = [Buf(), Buf()]
            R = [A.f32(512) for _ in range(2)]
            Rb = [A.bf(512) for _ in range(2)]
            BR = [Buf(), Buf()]
            BRb = [Buf(), Buf()]
            sets = [mkset(A, [("ra", "f", 256), ("rb", "f", 256), ("rc", "f", 256), ("rd", "f", 256), ("ro", "f", 512),
                              ("qkb", "b", 512), ("qkT", "b", 512), ("vb", "b", 512), ("PT", "b", 128),
                              ("tmpR0", "f", 512), ("tmpR1", "f", 512), ("sg", "f", 512), ("yn", "f", 512),
                              ("ybf", "b", 512), ("yT", "b", 512), ("st6", "f", 8)]) for _ in range(2)]
            wctx = {}

            def load_w(hh):
                gob, Bgo = gobs[hh % 2], Bgos[hh % 2]
                dma("sp", gob, W["c_g_out"][0, hh].partition_broadcast(128), [], [Bgo], next_io())
                sa = next_ws()
                wa = wslot[sa][:].rearrange("p (k n) -> p k n", k=8)
                wload(sa, 0, wa[:, :, 0:256], w_in[:, :, hh * 256:(hh + 1) * 256])
                wload(sa, 1, wa[:, :, 256:512], w_in[:, :, 1024 + hh * 256:1024 + (hh + 1) * 256])
                sv = next_ws()
                wv = wslot[sv][:].rearrange("p (k n) -> p k n", k=8)
                wload(sv, 0, wv, w_in[:, :, 2048 + hh * 512:2048 + (hh + 1) * 512])
                sg_ = next_ws()
                wgt = wslot[sg_][:].rearrange("p (k n) -> p k n", k=8)
                wload(sg_, 0, wgt, w_in[:, :, 4096 + hh * 512:4096 + (hh + 1) * 512])
                so = next_ws()
                wo = wslot[so][:].rearrange("p (c e) -> p c e", c=4)
                wload(so, 0, wo, W["c_w_out"][0, hh * 512:(hh + 1) * 512, :].rearrange("(c p) e -> p c e", p=128))
                wctx[hh] = (sa, wa, sv, wv, sg_, wgt, so, wo, gob, Bgo)

            def st0(it):
                hh, c = divmod(it, NT)
                sa, wa, sv, wv, sg_, wgt, so, wo, gob, Bgo = wctx[hh]
                T = sets[it % 2]
                ra, rb, rc, rd, vb, sg = [T[k] for k in ("ra", "rb", "rc", "rd", "vb", "sg")]
                Bra, Brb, Brc, Brd, Bvb, Bsg = [T["B" + k] for k in ("ra", "rb", "rc", "rd", "vb", "sg")]
                pqk, Bpqk = proj_tm(c, wa, 0, 512, Bws[sa])
                pv, Bpv = proj_tm(c, wv, 0, 512, Bws[sv])
                pg, Bpg = proj_tm(c, wgt, 0, 512, Bws[sg_])
                x4 = pqk[:].rearrange("p (q h i) -> p q h i", q=2, h=2)
                cb = bc_mid(cos3[:, c, :], 2)
                sb2 = bc_mid(sin3[:, c, :], 2)
                ra3 = ra.rearrange("p (q i) -> p q i", q=2)
                rb3 = rb.rearrange("p (q i) -> p q i", q=2)
                rc3 = rc.rearrange("p (q i) -> p q i", q=2)
                rd3 = rd.rearrange("p (q i) -> p q i", q=2)
                vtt(ra3, x4[:, :, 0, :], cb, ALU.mult, [Bpqk, Btab], [Bra])
                vtt(rb3, x4[:, :, 1, :], sb2, ALU.mult, [Bpqk, Btab], [Brb])
                vtt(rc3, x4[:, :, 1, :], cb, ALU.mult, [Bpqk, Btab], [Brc])
                vtt(rd3, x4[:, :, 0, :], sb2, ALU.mult, [Bpqk, Btab], [Brd])
                act(vb, pv[:], AF.Copy, [Bpv], [Bvb])
                act(sg, pg[:], AF.Silu, [Bpg], [Bsg])

            def st1(it):
                hh, c = divmod(it, NT)
                sa, wa, sv, wv, sg_, wgt, so, wo, gob, Bgo = wctx[hh]
                gch = RET_GCHUNK[hh]
                T = sets[it % 2]
                ra, rb, rc, rd, ro, qkb, qkT, vb, PT, sg, yn, ybf, yT, st6 = [T[k] for k in
                    ("ra", "rb", "rc", "rd", "ro", "qkb", "qkT", "vb", "PT", "sg", "yn", "ybf", "yT", "st6")]
                Bra, Brb, Brc, Brd, Bro, Bqkb, BqkT, Bvb, BPT, Bsg, Byn, Bybf, ByT, Bst = [T["B" + k] for k in
                    ("ra", "rb", "rc", "rd", "ro", "qkb", "qkT", "vb", "PT", "sg", "yn", "ybf", "yT", "st6")]
                if c == 0:
                    for d2 in range(2):
                        memset(R[d2], 0.0, [BR[d2]])
                        memset(Rb[d2], 0.0, [BRb[d2]])
                ra3 = ra.rearrange("p (q i) -> p q i", q=2)
                rb3 = rb.rearrange("p (q i) -> p q i", q=2)
                rc3 = rc.rearrange("p (q i) -> p q i", q=2)
                rd3 = rd.rearrange("p (q i) -> p q i", q=2)
                ro4 = ro.rearrange("p (q h i) -> p q h i", q=2, h=2)
                vtt(ro4[:, :, 0, :], ra3, rb3, ALU.subtract, [Bra, Brb], [Bro])
                vtt(ro4[:, :, 1, :], rc3, rd3, ALU.add, [Brc, Brd], [Bro])
                act(qkb[:, 0:256], ro[:, 0:256], AF.Copy, [Bro, Bconst], [Bqkb], scale=retc[:, hh:hh + 1])
                act(qkb[:, 256:512], ro[:, 256:512], AF.Copy, [Bro, Bconst], [Bqkb], scale=retc[:, 4 + hh:5 + hh])
                pb, Bpb = next_psb()
                for j in range(4):
                    tr(pb[:, j * 128:(j + 1) * 128], qkb[:, j * 128:(j + 1) * 128], [Bqkb], [Bpb])
                vcopy(qkT, pb[:, 0:512], [Bpb], [BqkT])
                pS, BpS = next_psf()
                for d2 in range(2):
                    mm(pS[:, 0:128], qkT[:, 256 + d2 * 128:256 + (d2 + 1) * 128], qkT[:, d2 * 128:(d2 + 1) * 128],
                       d2 == 0, d2 == 1, [BqkT], [BpS])
                vtt(PT, pS[:, 0:128], tri[:], ALU.mult, [BpS, Bconst], [BPT])
                py, Bpy = next_psf()
                mm(py[:], PT, vb, True, False, [BPT, Bvb], [Bpy])
                for d2 in range(2):
                    mm(py[:], qkT[:, d2 * 128:(d2 + 1) * 128], Rb[d2], False, d2 == 1, [BqkT, BRb[d2]], [Bpy])
                for d2 in range(2):
                    tmpR, BtR = T["tmpR%d" % d2], T["BtmpR%d" % d2]
                    pR, BpR = next_psf()
                    mm(pR[:], qkb[:, 256 + d2 * 128:256 + (d2 + 1) * 128], vb, True, True, [Bqkb, Bvb], [BpR])
                    vtt(tmpR, pR[:], R[d2], ALU.add, [BpR, BR[d2]], [BtR])
                    act(R[d2], tmpR, AF.Copy, [BtR], [BR[d2]], scale=gch)
                    act(Rb[d2], tmpR, AF.Copy, [BtR], [BRb[d2]], scale=gch)
                P.add("dve", lambda e, py=py, st6=st6: e.bn_stats(out=st6[:, 0:6], in_=py[:]), reads=[Bpy], writes=[Bst])
                P.add("dve", lambda e, st6=st6: e.bn_aggr(out=st6[:, 6:8], in_=st6[:, 0:6]), reads=[Bst], writes=[Bst])
                act(st6[:, 0:1], st6[:, 7:8], AF.Sqrt, [Bst], [Bst], bias=EPS)
                vrecip(st6[:, 1:2], st6[:, 0:1], [Bst], [Bst])
                vts(yn, py[:], st6[:, 6:7], st6[:, 1:2], ALU.subtract, ALU.mult, [Bpy, Bst], [Byn])
                vtt(sg, sg, gob, ALU.mult, [Bsg, Bgo], [Bsg])
                vtt(ybf, yn, sg, ALU.mult, [Byn, Bsg], [Bybf])
                pb2, Bpb2 = next_psb()
                for j in range(4):
                    tr(pb2[:, j * 128:(j + 1) * 128], ybf[:, j * 128:(j + 1) * 128], [Bybf], [Bpb2])
                vcopy(yT, pb2[:, 0:512], [Bpb2], [ByT])
                outproj_acc(c, [(yT[:, j * 128:(j + 1) * 128], ByT) for j in range(4)],
                            lambda cc, nh: wo[:, cc, nh * 512:(nh + 1) * 512], Bws[so])

            for hh in range(4):
                load_w(hh)
                pipeline(NT, [lambda c, hh=hh: st0(hh * NT + c), lambda c, hh=hh: st1(hh * NT + c)])

        def moba_phase(l):
            A = Arena()
            w_in = W["b_w_in"][0].rearrange("(kc p) n -> p kc n", p=128)
            cos_t = A.f32(NT * 16)
            sin_t = A.f32(NT * 16)
            Btab = Buf()
            mark = A.off
            trig_tables(A, invb[:], 16, cos_t, sin_t, Btab)
            A.off = mark
            cos3 = cos_t.rearrange("p (t i) -> p t i", i=16)
            sin3 = sin_t.rearrange("p (t i) -> p t i", i=16)
            P.barrier()
            gqk = A.f32(256)
            Bg = Buf()
            dma("sp", gqk[:, 0:128], W["b_g_q"][0].partition_broadcast(128), [], [Bg], next_io())
            dma("sp", gqk[:, 128:256], W["b_g_k"][0].partition_broadcast(128), [], [Bg], next_io())
            NBLK = S // 256
            hsets = []
            for _ in range(2):
                d = {"qT": A.bf(S), "kT": A.bf(S), "vaug": A.bf(NT * 130), "kmf": A.f32(8), "kmb": A.bf(8)}
                d["BqT"] = [Buf() for _ in range(NT)]
                d["BkT"] = [Buf() for _ in range(NT)]
                d["Bva"] = [Buf() for _ in range(NT)]
                d["Bkm"] = Buf()
                hsets.append(d)
            NPSET = 3
            psets = [mkset(A, [("qkn", "f", 256), ("r4", "f", 128), ("qkb", "b", 256), ("sm", "f", 8)])
                     for _ in range(NPSET)]
            NQS = 4
            qsets = [mkset(A, [("gm", "f", 8), ("m8", "f", 8), ("sel", "f", 8), ("acc", "f", 132), ("obf", "b", 128),
                               ("yTt", "b", 128)]) for _ in range(NQS)]
            NPT = 4
            ptsets = [mkset(A, [("PT", "b", 256)]) for _ in range(NPT)]
            sc = 128.0 ** -0.5
            for hh in range(8):
                H = hsets[hh % 2]
                qT, kT, kmf, kmb = H["qT"], H["kT"], H["kmf"], H["kmb"]
                va3 = H["vaug"].rearrange("p (t e) -> p t e", e=130)
                BqT, BkT, Bva, Bkm = H["BqT"], H["BkT"], H["Bva"], H["Bkm"]
                sa = next_ws()
                wa = wslot[sa][:, 0:3072].rearrange("p (k n) -> p k n", k=8)
                wload(sa, 0, wa[:, :, 0:128], w_in[:, :, hh * 128:(hh + 1) * 128])
                wload(sa, 0, wa[:, :, 128:256], w_in[:, :, 1024 + hh * 128:1024 + (hh + 1) * 128])
                wload(sa, 1, wa[:, :, 256:384], w_in[:, :, 2048 + hh * 128:2048 + (hh + 1) * 128])
                wo = wslot[sa][:, 3072:4096]
                wload(sa, 1, wo, W["b_w_out"][0, hh * 128:(hh + 1) * 128, :])
                pctx = {}

                def ip0(c):
                    T = psets[c % NPSET]
                    qkn, sm = T["qkn"], T["sm"]
                    Bqkn, Bs = T["Bqkn"], T["Bsm"]
                    p1, Bp1 = proj_tm(c, wa, 0, 384, Bws[sa])
                    act(scr[:, 0:128], p1[:, 0:128], AF.Square, [Bp1], [Bscr, Bs], accum_out=sm[:, 0:1])
                    act(scr[:, 128:256], p1[:, 128:256], AF.Square, [Bp1], [Bscr, Bs], accum_out=sm[:, 1:2])
                    act(va3[:, c, 0:128], p1[:, 256:384], AF.Copy, [Bp1], [Bva[c]])
                    act(sm[:, 2:4], sm[:, 0:2], AF.Sqrt, [Bs], [Bs], scale=1.0 / 128, bias=EPS)
                    vrecip(sm[:, 4:6], sm[:, 2:4], [Bs], [Bs])
                    vstt(qkn[:, 0:128], p1[:, 0:128], sm[:, 4:5], gqk[:, 0:128], ALU.mult, ALU.mult,
                         [Bp1, Bs, Bg], [Bqkn])
                    vstt(qkn[:, 128:256], p1[:, 128:256], sm[:, 5:6], gqk[:, 128:256], ALU.mult, ALU.mult,
                         [Bp1, Bs, Bg], [Bqkn])
                    memset(va3[:, c, 128:129], 1.0, [Bva[c]])

                def ip1(c):
                    T = psets[c % NPSET]
                    qkn, r4, qkb = T["qkn"], T["r4"], T["qkb"]
                    Bqkn, Br4, Bqkb = T["Bqkn"], T["Br4"], T["Bqkb"]
                    q3 = qkn.rearrange("p (q d) -> p q d", q=2)
                    x1 = q3[:, :, 0:16]
                    x2 = q3[:, :, 16:32]
                    cb = bc_mid(cos3[:, c, :], 2)
                    sb2 = bc_mid(sin3[:, c, :], 2)
                    r43 = r4.rearrange("p (a q i) -> p a q i", a=4, q=2)
                    vtt(r43[:, 0], x1, cb, ALU.mult, [Bqkn, Btab], [Br4])
                    vtt(r43[:, 1], x2, sb2, ALU.mult, [Bqkn, Btab], [Br4])
                    vtt(r43[:, 2], x2, cb, ALU.mult, [Bqkn, Btab], [Br4])
                    vtt(r43[:, 3], x1, sb2, ALU.mult, [Bqkn, Btab], [Br4])
                    vtt(x1, r43[:, 0], r43[:, 1], ALU.subtract, [Br4], [Bqkn])
                    vtt(x2, r43[:, 2], r43[:, 3], ALU.add, [Br4], [Bqkn])
                    act(qkb[:, 0:128], qkn[:, 0:128], AF.Copy, [Bqkn], [Bqkb], scale=sc)
                    act(qkb[:, 128:256], qkn[:, 128:256], AF.Copy, [Bqkn], [Bqkb])
                    pb, Bpb = next_psb()
                    tr(pb[:, 0:128], qkb[:, 0:128], [Bqkb], [Bpb])
                    tr(pb[:, 128:256], qkb[:, 128:256], [Bqkb], [Bpb])
                    vcopy(qT[:, c * 128:(c + 1) * 128], pb[:, 0:128], [Bpb], [BqT[c]])
                    vcopy(kT[:, c * 128:(c + 1) * 128], pb[:, 128:256], [Bpb], [BkT[c]])

                pipeline(NT, [ip0, ip1])
                P.add("dve", lambda e, kmf=kmf, kT=kT: e.tensor_reduce(
                    out=kmf[:, 0:NBLK], in_=kT.rearrange("p (b n) -> p b n", n=256), axis=AX.X, op=ALU.add),
                      reads=BkT, writes=[Bkm])
                act(kmb[:, 0:NBLK], kmf[:, 0:NBLK], AF.Copy, [Bkm], [Bkm], scale=1.0 / 256)
                items = [(qi, b) for qi in range(NT) for b in range(qi // 2 + 1)]
                ictx = {}

                def at0(k):
                    qi, b = items[k]
                    j = qi // 2
                    own = (b == j)
                    chunks = [0] if (own and qi % 2 == 0) else [0, 1]
                    pS, BpS = next_psf()
                    for kc in chunks:
                        kt = b * 2 + kc
                        mm(pS[:, kc * 128:(kc + 1) * 128], kT[:, kt * 128:(kt + 1) * 128],
                           qT[:, qi * 128:(qi + 1) * 128], True, True, [BkT[kt], BqT[qi]], [BpS])
                    PTs = ptsets[k % NPT]
                    PT, BPT = PTs["PT"], PTs["BPT"]
                    n = len(chunks) * 128
                    act(PT[:, 0:n], pS[:, 0:n], AF.Exp, [BpS], [BPT])
                    if own:
                        dg = qi % 2
                        vtt(PT[:, dg * 128:(dg + 1) * 128], PT[:, dg * 128:(dg + 1) * 128], tribf[:], ALU.mult,
                            [BPT, Bconst], [BPT])
                    ictx[k] = chunks

                def at1(k):
                    qi, b = items[k]
                    j = qi // 2
                    nprev = j
                    own = (b == j)
                    chunks = ictx.pop(k)
                    Q = qsets[qi % NQS]
                    gm, m8, sel, acc, obf, yTt = Q["gm"], Q["m8"], Q["sel"], Q["acc"], Q["obf"], Q["yTt"]
                    Bgm, Bsel, Bacc, Bobf, ByT = Q["Bgm"], Q["Bsel"], Q["Bacc"], Q["Bobf"], Q["ByTt"]
                    PTs = ptsets[k % NPT]
                    PT, BPT = PTs["PT"], PTs["BPT"]
                    if b == 0 and nprev > 3:
                        pgt, Bpgt = next_psf()
                        mm(pgt[:, 0:NBLK], qT[:, qi * 128:(qi + 1) * 128], kmb[:, 0:NBLK], True, True,
                           [BqT[qi], Bkm], [Bpgt])
                        memset(gm, -1e30, [Bgm])
                        vcopy(gm[:, 0:nprev], pgt[:, 0:nprev], [Bpgt], [Bgm])
                        P.add("dve", lambda e, m8=m8, gm=gm: e.max(out=m8, in_=gm), reads=[Bgm], writes=[Bgm])
                        vts(sel, gm, m8[:, 2:3], None, ALU.is_ge, None, [Bgm], [Bsel])
                    pO, BpO = next_psf()
                    for ci, kc in enumerate(chunks):
                        kt = b * 2 + kc
                        mm(pO[:, 0:129], PT[:, kc * 128:(kc + 1) * 128], va3[:, kt, 0:129], ci == 0,
                           ci == len(chunks) - 1, [BPT, Bva[kt]], [BpO])
                    use_sel = (not own) and nprev > 3
                    if b == 0:
                        if use_sel:
                            vts(acc[:, 0:129], pO[:, 0:129], sel[:, b:b + 1], None, ALU.mult, None,
                                [BpO, Bsel], [Bacc])
                        else:
                            vcopy(acc[:, 0:129], pO[:, 0:129], [BpO], [Bacc])
                    else:
                        if use_sel:
                            vstt(acc[:, 0:129], pO[:, 0:129], sel[:, b:b + 1], acc[:, 0:129], ALU.mult, ALU.add,
                                 [BpO, Bsel, Bacc], [Bacc])
                        else:
                            vtt(acc[:, 0:129], pO[:, 0:129], acc[:, 0:129], ALU.add, [BpO, Bacc], [Bacc])
                    if own:
                        vrecip(acc[:, 130:131], acc[:, 128:129], [Bacc], [Bacc])
                        act(obf, acc[:, 0:128], AF.Copy, [Bacc], [Bobf], scale=acc[:, 130:131])

                def at2(k):
                    qi, b = items[k]
                    if b != qi // 2:
                        return
                    Q = qsets[qi % NQS]
                    obf, yTt, Bobf, ByT = Q["obf"], Q["yTt"], Q["Bobf"], Q["ByTt"]
                    pb2, Bpb2 = next_psb()
                    tr(pb2[:, 0:128], obf, [Bobf], [Bpb2])
                    vcopy(yTt, pb2[:, 0:128], [Bpb2], [ByT])
                    outproj_acc(qi, [(yTt, ByT)], lambda cc, nh: wo[:, nh * 512:(nh + 1) * 512], Bws[sa])

                def nop_stage(k):
                    pass

                pipeline(len(items), [at0, nop_stage, at1, nop_stage, at2])

        MIXERS = {0: mlstm_phase, 1: moba_phase, 2: retention_phase, 3: rglru_phase}

        for s in range(nseq):
            for t in range(NT):
                dma("sp", h[:, t, :], x_d[s, t * 128:(t + 1) * 128, :], [], [Bh[t]], next_io())
            dma("sp", posi[:], pos_d[s], [], [Bpos], next_io())
            vcopy(posf[:], posi[:], [Bpos], [Bpos])
            for kind, l in plan:
                if kind == "mix":
                    norm_phase(W["norm_mix"][l])
                    P.barrier()
                    MIXERS[l](l)
                else:
                    norm_phase(W["norm_ffn"][l])
                    P.barrier()
                    ffn_phase(l)
            outs = []
            for t in range(NT):
                outs.append(dma("sp", out_d[s, t * 128:(t + 1) * 128, :], h[:, t, :], [Bh[t]], [], next_io()))
            P.add("sp", None, extra_deps=[sl.last for sl in io_slots])
        P.finalize(sems)
        with nc.Block() as block:
            P.emit(block)
    return nc


def kernel(**inputs):
    ncores = 8
    x = np.ascontiguousarray(inputs["x"], dtype=np.float32)
    pos = np.asarray(inputs["positions"], dtype=np.int32)
    pos = np.ascontiguousarray(pos.reshape(pos.shape[0], -1, 128).transpose(0, 2, 1))
    B = x.shape[0]
    per = B // ncores
    nc = build(nseq=per, S=x.shape[1])
    consts = host_consts()
    in_maps = []
    for c in range(ncores):
        m = {"x": x[c * per:(c + 1) * per], "positions": pos[c * per:(c + 1) * per]}
        for k in DEV_PARAMS:
            m[k] = np.ascontiguousarray(inputs[k], dtype=np.float32)
        m["d_small"] = pack_d_small(inputs)
        m.update(consts)
        in_maps.append(m)
    res = run_bass_kernel_spmd(nc, in_maps, core_ids=list(range(ncores)))
    return np.concatenate([r["out"] for r in res.results], axis=0).astype(np.float32)
```

```python
import contextlib
import math
import os
import numpy as np
import ml_dtypes
import concourse.bass as bass
import concourse.mybir as mybir
from concourse.bass_utils import run_bass_kernel_spmd

F32 = mybir.dt.float32
BF16 = mybir.dt.bfloat16
I32 = mybir.dt.int32
AF = mybir.ActivationFunctionType
ALU = mybir.AluOpType
AX = mybir.AxisListType

D = 1024
DFF = 2816
EPS = 1e-6
ENGS = ("pe", "act", "dve", "pool", "sp")


_ARENA = [False]


class Buf:
    __slots__ = ("name", "w", "r", "rd", "excl", "arena")

    def __init__(self, name="", excl=False):
        self.name = name
        self.arena = _ARENA[0]
        self.w = None
        self.r = []
        self.rd = []
        self.excl = excl


class Slot:
    __slots__ = ("sem", "count", "last")

    def __init__(self, sem):
        self.sem = sem
        self.count = 0
        self.last = None


class Op:
    __slots__ = ("eng", "fn", "deps", "pos", "sig", "sigidx", "waits", "slot", "slotval", "cost", "idx",
                 "succ", "nin", "ready", "fin", "barrier_slots")

    def __init__(self, eng, fn):
        self.eng = eng
        self.fn = fn
        self.deps = {}
        self.pos = -1
        self.sig = False
        self.sigidx = 0
        self.waits = []
        self.slot = None
        self.slotval = 0
        self.cost = 100.0
        self.barrier_slots = None


class Prog:
    def __init__(self):
        self.ops = {e: [] for e in ENGS}
        self.all = []
        self.slots = []
        self.epoch = Buf("arena_epoch")

    def fence(self):
        return self.add("sp", lambda e: e.nop(), writes=[self.epoch], cost=30.0)

    def add(self, eng, fn, reads=(), writes=(), slot=None, extra_deps=(), cost=100.0):
        op = Op(eng, fn)
        op.cost = cost
        deps = op.deps
        for b in reads:
            if b.arena:
                reads = list(reads) + [self.epoch]
                break
        else:
            for b in writes:
                if b.arena:
                    reads = list(reads) + [self.epoch]
                    break
        for b in reads:
            if b.w is not None:
                deps[b.w] = "raw"
            if b.excl:
                for r in b.r:
                    if r.eng != eng:
                        deps.setdefault(r, "war")
        for b in writes:
            if b.w is not None:
                deps.setdefault(b.w, "waw")
            for r in b.r:
                deps.setdefault(r, "war")
            for r in b.rd:
                deps.setdefault(r, "war")
        for d in extra_deps:
            if d is not None:
                deps[d] = "raw"
        if slot is not None:
            if slot.last is not None:
                deps[slot.last] = "raw"
            slot.count += 1
            op.slot = slot
            op.slotval = 16 * slot.count
            slot.last = op
        deps.pop(op, None)
        for b in writes:
            b.w = op
            b.r = []
            b.rd = []
        for b in reads:
            if slot is not None:
                b.rd.append(op)
            else:
                b.r.append(op)
        self.all.append(op)
        return op

    def barrier(self):
        b1 = Op("sp", lambda e: e.nop())
        b1.barrier_slots = [s.last for s in self.slots if s.last is not None]
        self.all.append(b1)
        return b1

    @staticmethod
    def _schedule(seg):
        import heapq
        inseg = set(seg)
        for i, op in enumerate(seg):
            op.idx = i
            op.succ = []
            op.nin = 0
            op.ready = 0.0
        for op in seg:
            for d in op.deps:
                if d in inseg:
                    d.succ.append(op)
                    op.nin += 1
        heap = [(0.0, op.idx, op) for op in seg if op.nin == 0]
        heapq.heapify(heap)
        free = {e: 0.0 for e in ENGS}
        order = []
        while heap:
            rdy, _, op = heapq.heappop(heap)
            start = max(rdy, free[op.eng])
            if op.slot is not None:
                free[op.eng] = start + 120.0
                fin = start + op.cost
            else:
                fin = start + op.cost
                free[op.eng] = fin
            order.append(op)
            for s_ in op.succ:
                lat = 60.0 if (s_.eng == op.eng and op.slot is None) else 220.0
                t = fin + lat
                if t > s_.ready:
                    s_.ready = t
                s_.nin -= 1
                if s_.nin == 0:
                    heapq.heappush(heap, (s_.ready, s_.idx, s_))
        assert len(order) == len(seg)
        return order

    def finalize(self, sems, schedule=True):
        segs = [[]]
        bars = []
        for op in self.all:
            if op.barrier_slots is not None:
                bars.append(op)
                segs.append([])
            else:
                segs[-1].append(op)
        newall = []
        self.ops = {e: [] for e in ENGS}
        for si, seg in enumerate(segs):
            order = self._schedule(seg) if schedule else seg
            for op in order:
                op.pos = len(self.ops[op.eng])
                self.ops[op.eng].append(op)
                newall.append(op)
            if si < len(bars):
                b1 = bars[si]
                for e in ENGS:
                    for op in reversed(self.ops[e]):
                        if op.fn is not None:
                            b1.deps[op] = "raw"
                            break
                for d in b1.barrier_slots:
                    b1.deps[d] = "raw"
                b1.pos = len(self.ops["sp"])
                self.ops["sp"].append(b1)
                newall.append(b1)
                for e in ENGS:
                    if e != "sp":
                        w = Op(e, None)
                        w.deps[b1] = "raw"
                        w.pos = len(self.ops[e])
                        self.ops[e].append(w)
                        newall.append(w)
        self.all = newall
        known = {x: {y: -1 for y in ENGS} for x in ENGS}
        knownslot = {x: {} for x in ENGS}
        for op in self.all:
            x = op.eng
            for dep, kind in op.deps.items():
                if dep.slot is not None:
                    if knownslot[x].get(dep.slot, 0) >= dep.slotval:
                        continue
                    knownslot[x][dep.slot] = dep.slotval
                    op.waits.append((dep.slot.sem, dep.slotval))
                else:
                    y = dep.eng
                    if y == x and x == "pe":
                        assert dep.pos < op.pos
                        continue
                    if known[x][y] >= dep.pos:
                        continue
                    known[x][y] = dep.pos
                    dep.sig = True
                    op.waits.append((y, dep))
        for e in ENGS:
            c = 0
            for op in self.ops[e]:
                if op.sig:
                    c += 1
                    op.sigidx = c
        self.sems = sems

    def emit_engine(self, ename, eng):
        sems = self.sems
        for op in self.ops[ename]:
            for w in op.waits:
                if isinstance(w[0], str):
                    eng.wait_ge(sems[w[0]], w[1].sigidx)
                else:
                    eng.wait_ge(w[0], w[1])
            if op.fn is None:
                assert not op.sig
                continue
            inst = op.fn(eng)
            if op.slot is not None:
                inst.then_inc(op.slot.sem, 16)
            elif op.sig:
                inst.then_inc(sems[ename], 1)

    def emit(self, block):
        block.tensor(lambda e: self.emit_engine("pe", e))
        block.scalar(lambda e: self.emit_engine("act", e))
        block.vector(lambda e: self.emit_engine("dve", e))
        block.gpsimd(lambda e: self.emit_engine("pool", e))
        block.sync(lambda e: self.emit_engine("sp", e))


PARAM_SHAPES = {
    "norm_mix": [4, 1024], "norm_ffn": [4, 1024],
    "ffn_w_in": [4, 1024, 5632], "ffn_w_out": [4, 2816, 1024],
    "a_w_in": [1, 1024, 3080], "a_b_gates": [1, 8], "a_g_out": [1, 4, 256], "a_w_out": [1, 1024, 1024],
    "b_w_in": [1, 1024, 3072], "b_g_q": [1, 128], "b_g_k": [1, 128], "b_w_out": [1, 1024, 1024],
    "c_w_in": [1, 1024, 6144], "c_g_out": [1, 4, 512], "c_w_out": [1, 2048, 1024],
    "d_w_in": [1, 1024, 2048], "d_conv_w": [1, 4, 1024], "d_conv_b": [1, 1024],
    "d_w_gates": [1, 4, 256, 512], "d_b_gates": [1, 4, 512], "d_lru": [1, 1024], "d_w_out": [1, 1024, 1024],
}
DEV_PARAMS = [k for k in PARAM_SHAPES if k not in ("d_conv_w", "d_conv_b", "d_b_gates", "d_lru")]


def pack_d_small(inputs):
    cw = np.asarray(inputs["d_conv_w"], np.float32)[0].reshape(4, 8, 128).transpose(2, 0, 1).reshape(128, 32)
    cb = np.asarray(inputs["d_conv_b"], np.float32)[0].reshape(8, 128).T
    bg = np.asarray(inputs["d_b_gates"], np.float32)[0].reshape(4, 4, 128).transpose(2, 0, 1).reshape(128, 16)
    lr = np.asarray(inputs["d_lru"], np.float32)[0].reshape(8, 128).T
    return np.ascontiguousarray(np.concatenate([cw, cb, bg, lr], axis=1))


def host_consts():
    c = {}
    c["k_ident"] = np.eye(128, dtype=np.float32).astype(ml_dtypes.bfloat16)
    tri = (np.arange(128)[:, None] <= np.arange(128)[None, :]).astype(np.float32)
    c["k_tri"] = tri
    c["k_tribf"] = tri.astype(ml_dtypes.bfloat16)
    c["k_ones"] = np.ones((128, 128), np.float32)
    invb = (500000.0 ** (-np.arange(16, dtype=np.float32) * (2.0 / 32))).astype(np.float32)
    c["k_invb"] = np.broadcast_to(invb[None, :], (128, 16)).copy()
    invc = (10000.0 ** (-np.arange(128, dtype=np.float32) * (2.0 / 256))).astype(np.float32)
    c["k_invc"] = np.broadcast_to(invc[None, :], (128, 128)).copy()
    ret = np.zeros((128, 8), np.float64)
    p = np.arange(128, dtype=np.float64)
    for hh in range(4):
        lg = np.log1p(-np.exp2(-5.0 - hh))
        ret[:, hh] = np.exp((p + 1.0) * lg)
        ret[:, 4 + hh] = np.exp(-(p + 1.0) * lg) * (256 ** -0.5)
    c["k_ret"] = ret.astype(np.float32)
    return c


CONST_SPECS = {"k_ident": ([128, 128], BF16), "k_tri": ([128, 128], F32), "k_tribf": ([128, 128], BF16),
               "k_ones": ([128, 128], F32), "k_invb": ([128, 16], F32), "k_invc": ([128, 128], F32),
               "k_ret": ([128, 8], F32)}
RET_GCHUNK = [float(np.exp(128.0 * np.log1p(-np.exp2(-5.0 - hh)))) for hh in range(4)]


def build(nseq=2, S=2048, plan=None):
    if plan is None:
        plan = [("mix", l) if k == 0 else ("ffn", l) for l in range(4) for k in range(2)]
    NT = S // 128
    NG = S // 512
    nc = bass.Bass("TRN2", target_bir_lowering=False)
    x_d = nc.dram_tensor("x", [nseq, S, D], F32, kind="ExternalInput").ap()
    pos_d = nc.dram_tensor("positions", [nseq, 128, S // 128], I32, kind="ExternalInput").ap()
    W = {k: nc.dram_tensor(k, PARAM_SHAPES[k], F32, kind="ExternalInput").ap() for k in DEV_PARAMS}
    W["d_small"] = nc.dram_tensor("d_small", [128, 64], F32, kind="ExternalInput").ap()
    KC = {k: nc.dram_tensor(k, v[0], v[1], kind="ExternalInput").ap() for k, v in CONST_SPECS.items()}
    out_d = nc.dram_tensor("out", [nseq, S, D], F32, kind="ExternalOutput").ap()

    P = Prog()
    with contextlib.ExitStack() as st:
        def sb(name, shape, dt):
            return st.enter_context(nc.sbuf_tensor(name, shape, dt))

        def pst(name, shape, dt):
            return st.enter_context(nc.psum_tensor(name, shape, dt))

        def newsem(name):
            return st.enter_context(nc.semaphore(name))

        sems = {e: newsem("s_" + e) for e in ENGS}
        nslots = [0]

        def newslot():
            s = Slot(newsem("d%d" % nslots[0]))
            nslots[0] += 1
            P.slots.append(s)
            return s

        h = sb("h", [128, NT, D], F32)
        Bh = [Buf("h%d" % t) for t in range(NT)]
        uT = sb("uT", [128, 8, S], BF16)
        BuT = [Buf("uT%d" % t) for t in range(NT)]
        NWS = 4
        wslot = [sb("ws%d" % i, [128, 4096], BF16) for i in range(NWS)]
        Bws = [Buf("ws%d" % i) for i in range(NWS)]
        ws_dslots = [(newslot(), newslot()) for _ in range(NWS)]
        ws_rr = [0]
        gbc = sb("gbc", [128, D], F32)
        Bgbc = Buf("gbc")
        gbc_slot = newslot()
        ARENA = 16640
        arena = sb("arena", [128, ARENA], F32)
        arena_bf = arena[:].bitcast(BF16)
        scr = sb("scr", [128, D], BF16)
        Bscr = Buf("scr")
        ubf = [sb("ubf%d" % i, [128, D], BF16) for i in range(2)]
        Bubf = [Buf("ubf%d" % i) for i in range(2)]
        stat = sb("stat", [128, 3 * NT], F32)
        Bstat = [Buf("stat%d" % t) for t in range(NT)]
        ident = sb("ident", [128, 128], BF16)
        tri = sb("tri", [128, 128], F32)
        tribf = sb("tribf", [128, 128], BF16)
        ones = sb("ones", [128, 128], F32)
        invb = sb("invb", [128, 16], F32)
        invc = sb("invc", [128, 128], F32)
        retc = sb("retc", [128, 8], F32)
        neghalf = sb("neghalf", [128, 2], F32)
        Bconst = Buf("const")
        posi = sb("posi", [128, NT], I32)
        posf = sb("posf", [128, NT], F32)
        Bpos = Buf("pos")

        NPS = 6
        psf = [pst("psf%d" % i, [128, 512], F32) for i in range(NPS)]
        Bpsf = [Buf("psf%d" % i, excl=True) for i in range(NPS)]
        psb = [pst("psb%d" % i, [128, 1024], BF16) for i in range(2)]
        Bpsb = [Buf("psb%d" % i, excl=True) for i in range(2)]
        rr = {"psf": 0, "psb": 0, "io": 0, "ubf": 0}

        def next_psf():
            i = rr["psf"]
            rr["psf"] = (i + 1) % NPS
            return psf[i], Bpsf[i]

        def next_psb():
            i = rr["psb"]
            rr["psb"] = (i + 1) % 2
            return psb[i], Bpsb[i]

        io_slots = [newslot() for _ in range(4)]

        def next_io():
            i = rr["io"]
            rr["io"] = (i + 1) % 4
            return io_slots[i]

        def next_ws():
            i = ws_rr[0]
            ws_rr[0] = (i + 1) % NWS
            return i

        def fsz(ap):
            n = 1
            for d in ap.shape[1:]:
                n *= int(d)
            return n

        def dma(eng, out, in_, reads, writes, slot):
            nbytes = fsz(out) * 128 * 4
            return P.add(eng, lambda e: e.dma_start(out=out, in_=in_), reads=reads, writes=writes, slot=slot,
                         cost=2500.0 + nbytes / 150.0)

        def wload(i, part, out, in_):
            return dma("pool", out, in_, [], [Bws[i]], ws_dslots[i][part])

        def mm(out, lhsT, rhs, start, stop, reads, writes):
            fp32 = (lhsT.dtype == F32)
            return P.add("pe", lambda e: e.matmul(out, lhsT=lhsT, rhs=rhs, start=start, stop=stop),
                         reads=reads, writes=writes, cost=max(60.0, fsz(out) * (0.5 if not fp32 else 2.0)) + 10.0)

        def tr(out, in_, reads, writes):
            return P.add("pe", lambda e: e.transpose(out=out, in_=in_, identity=ident[:]),
                         reads=list(reads) + [Bconst], writes=writes, cost=65.0)

        def act(out, in_, func, reads, writes, **kw):
            return P.add("act", lambda e: e.activation(out=out, in_=in_, func=func, **kw), reads=reads, writes=writes,
                         cost=230.0 + fsz(out) * 0.85 + (100.0 if "accum_out" in kw else 0.0))

        def vtt(out, in0, in1, op, reads, writes, eng="dve"):
            return P.add(eng, lambda e: e.tensor_tensor(out=out, in0=in0, in1=in1, op=op), reads=reads, writes=writes,
                         cost=70.0 + fsz(out) * 1.2)

        def vts(out, in0, s1, s2, op0, op1, reads, writes, eng="dve"):
            if op1 is None:
                return P.add(eng, lambda e: e.tensor_scalar(out=out, in0=in0, scalar1=s1, scalar2=None, op0=op0),
                             reads=reads, writes=writes, cost=70.0 + fsz(out) * 0.7)
            return P.add(eng, lambda e: e.tensor_scalar(out=out, in0=in0, scalar1=s1, scalar2=s2, op0=op0, op1=op1),
                         reads=reads, writes=writes, cost=70.0 + fsz(out) * 0.7)

        def vstt(out, in0, scalar, in1, op0, op1, reads, writes):
            return P.add("dve", lambda e: e.scalar_tensor_tensor(out=out, in0=in0, scalar=scalar, in1=in1,
                                                                 op0=op0, op1=op1), reads=reads, writes=writes,
                         cost=70.0 + fsz(out) * 1.1)

        def vcopy(out, in_, reads, writes, eng="dve"):
            return P.add(eng, lambda e: e.tensor_copy(out=out, in_=in_), reads=reads, writes=writes,
                         cost=70.0 + fsz(out) * 1.0)

        def vrecip(out, in_, reads, writes):
            return P.add("dve", lambda e: e.reciprocal(out=out, in_=in_), reads=reads, writes=writes,
                         cost=70.0 + fsz(out) * 8.0)

        def memset(ap, val, writes, eng="dve"):
            return P.add(eng, lambda e: e.memset(ap, val), writes=writes)

        for name, t in (("k_ident", ident), ("k_tri", tri), ("k_tribf", tribf), ("k_ones", ones),
                        ("k_invb", invb), ("k_invc", invc), ("k_ret", retc)):
            dma("sp", t[:], KC[name], [], [Bconst], next_io())


        def norm_phase(g_row):
            dma("sp", gbc[:], g_row.partition_broadcast(128), [], [Bgbc], gbc_slot)
            for t in range(NT):
                ss = stat[:, 3 * t:3 * t + 1]
                sq = stat[:, 3 * t + 1:3 * t + 2]
                rs = stat[:, 3 * t + 2:3 * t + 3]
                act(scr[:], h[:, t, :], AF.Square, [Bh[t]], [Bscr, Bstat[t]], accum_out=ss)
                act(sq, ss, AF.Sqrt, [Bstat[t]], [Bstat[t]], scale=1.0 / D, bias=EPS)
                vrecip(rs, sq, [Bstat[t]], [Bstat[t]])
                ui = rr["ubf"]
                rr["ubf"] = 1 - ui
                vstt(ubf[ui][:], h[:, t, :], rs, gbc[:], ALU.mult, ALU.mult, [Bh[t], Bstat[t], Bgbc], [Bubf[ui]])
                pb, Bpb = next_psb()
                for k in range(8):
                    tr(pb[:, k * 128:(k + 1) * 128], ubf[ui][:, k * 128:(k + 1) * 128], [Bubf[ui]], [Bpb])
                vcopy(uT[:, :, t * 128:(t + 1) * 128], pb[:].rearrange("p (k c) -> p k c", k=8), [Bpb], [BuT[t]])

        def ffn_phase(l):
            w_in = W["ffn_w_in"][l].rearrange("(kc p) n -> p kc n", p=128)
            w_out = W["ffn_w_out"][l]
            hT = arena_bf[:, 0:6 * S].rearrange("p (c s) -> p c s", c=6)
            BhT = [[Buf() for _ in range(NG)] for _ in range(6)]
            wo = [arena_bf[:, 6 * S + i * 6144: 6 * S + (i + 1) * 6144].rearrange("p (c n) -> p c n", c=6)
                  for i in range(2)]
            Bwo = [Buf(), Buf()]
            wo_slot = [newslot(), newslot()]
            sg = [arena[:, ARENA - 1024 + i * 512: ARENA - 1024 + (i + 1) * 512] for i in range(2)]
            assert 6 * S + 2 * 6144 <= 2 * ARENA - 2048
            Bsg = [Buf(), Buf()]
            sgi = 0
            quarters = [[0, 1, 2], [3, 4, 5], [6, 7, 8], [9, 10]]
            for qi, groups in enumerate(quarters):
                nch = 2 * len(groups)
                r0 = groups[0] * 256
                wi = qi % 2
                dma("pool", wo[wi][:, 0:nch, :], w_out[r0:r0 + nch * 128, :].rearrange("(c p) n -> p c n", p=128),
                    [], [Bwo[wi]], wo_slot[wi])
                for gi, g in enumerate(groups):
                    si = next_ws()
                    wsv = wslot[si][:].rearrange("p (k n) -> p k n", k=8)
                    wload(si, 0, wsv[:, :, 0:256], w_in[:, :, g * 256:(g + 1) * 256])
                    wload(si, 1, wsv[:, :, 256:512], w_in[:, :, DFF + g * 256:DFF + (g + 1) * 256])
                    for half in range(2):
                        ch = gi * 2 + half
                        for tg in range(NG):
                            pg, Bpg = next_psf()
                            pu, Bpu = next_psf()
                            ur = [BuT[tg * 4 + i] for i in range(4)]
                            for kc in range(8):
                                mm(pg[:], wsv[:, kc, half * 128:(half + 1) * 128], uT[:, kc, tg * 512:(tg + 1) * 512],
                                   kc == 0, kc == 7, [Bws[si]] + ur, [Bpg])
                            for kc in range(8):
                                mm(pu[:], wsv[:, kc, 256 + half * 128:256 + (half + 1) * 128],
                                   uT[:, kc, tg * 512:(tg + 1) * 512], kc == 0, kc == 7, [Bws[si]] + ur, [Bpu])
                            act(sg[sgi][:], pg[:], AF.Silu, [Bpg], [Bsg[sgi]])
                            vtt(hT[:, ch, tg * 512:(tg + 1) * 512], pu[:], sg[sgi][:], ALU.mult,
                                [Bpu, Bsg[sgi]], [BhT[ch][tg]])
                            sgi = 1 - sgi
                for t in range(NT):
                    for nh in range(2):
                        po, Bpo = next_psf()
                        for c in range(nch):
                            mm(po[:], hT[:, c, t * 128:(t + 1) * 128], wo[wi][:, c, nh * 512:(nh + 1) * 512],
                               c == 0, c == nch - 1, [BhT[c][t // 4], Bwo[wi]], [Bpo])
                        vtt(h[:, t, nh * 512:(nh + 1) * 512], po[:], h[:, t, nh * 512:(nh + 1) * 512], ALU.add,
                            [Bpo, Bh[t]], [Bh[t]])

        class Arena:
            def __init__(self):
                self.off = 0

            def f32(self, n):
                o = self.off
                self.off += n
                assert self.off <= ARENA, self.off
                return arena[:, o:o + n]

            def bf(self, n):
                o = self.off
                self.off += (n + 1) // 2
                assert self.off <= ARENA, self.off
                return arena_bf[:, 2 * o:2 * o + n]

            def i32(self, n):
                return self.f32(n).bitcast(I32)

        def mkset(A, spec):
            d = {}
            for name, kind, n in spec:
                d[name] = A.f32(n) if kind == "f" else A.bf(n)
                d["B" + name] = Buf()
            return d

        def bc_mid(ap2d, n):
            return ap2d.unsqueeze(1).broadcast_to([ap2d.shape[0], n, ap2d.shape[1]])

        def proj_tm(t, wsv, c0, n, Bw):
            ps, Bp = next_psf()
            for kc in range(8):
                mm(ps[:, 0:n], uT[:, kc, t * 128:(t + 1) * 128], wsv[:, kc, c0:c0 + n], kc == 0, kc == 7,
                   [BuT[t], Bw], [Bp])
            return ps, Bp

        def proj_fm(tg, wsv, c0, Bw):
            ps, Bp = next_psf()
            ur = [BuT[tg * 4 + i] for i in range(4)]
            for kc in range(8):
                mm(ps[:], wsv[:, kc, c0:c0 + 128], uT[:, kc, tg * 512:(tg + 1) * 512], kc == 0, kc == 7,
                   [Bw] + ur, [Bp])
            return ps, Bp

        def outproj_acc(t, lhs_list, wfn, Bw):
            n = len(lhs_list)
            for nh in range(2):
                po, Bpo = next_psf()
                for c, (lap, lb) in enumerate(lhs_list):
                    mm(po[:], lap, wfn(c, nh), c == 0, c == n - 1, [lb, Bw], [Bpo])
                vtt(h[:, t, nh * 512:(nh + 1) * 512], po[:], h[:, t, nh * 512:(nh + 1) * 512], ALU.add,
                    [Bpo, Bh[t]], [Bh[t]])

        def trig_tables(A, inv_tile, ni, cos_out, sin_out, Bt):
            n = NT * ni
            ang = A.f32(n)
            angs = A.f32(n)
            kf = A.f32(n)
            ki = A.i32(n)
            Bl = Buf()
            ang3 = ang.rearrange("p (t i) -> p t i", i=ni)
            for t in range(NT):
                vts(ang3[:, t, :], inv_tile, posf[:, t:t + 1], None, ALU.mult, None, [Bconst, Bpos], [Bl])
            PI = math.pi
            for shift, out in ((0.0, sin_out), (PI / 2, cos_out)):
                vts(angs, ang, shift, None, ALU.add, None, [Bl], [Bl])
                vts(kf, angs, 1.0 / (2 * PI), None, ALU.mult, None, [Bl], [Bl])
                vcopy(ki, kf, [Bl], [Bl])
                vcopy(kf, ki, [Bl], [Bl])
                vstt(angs, kf, -2 * PI, angs, ALU.mult, ALU.add, [Bl], [Bl])
                vts(angs, angs, -3.14159, 3.14159, ALU.max, ALU.min, [Bl], [Bl])
                act(out, angs, AF.Sin, [Bl], [Bt, Bl])

        def pipeline(n, stages):
            ns = len(stages)
            for k in range(n + ns - 1):
                for s_, f in enumerate(stages):
                    i = k - s_
                    if 0 <= i < n:
                        f(i)

        def rglru_phase(l):
            A = Arena()
            w_in = W["d_w_in"][0].rearrange("(kc p) n -> p kc n", p=128)
            small = A.f32(64)
            Bsm = Buf()
            dma("sp", small, W["d_small"], [], [Bsm], next_io())
            cw = small[:, 0:32]
            cb = small[:, 32:40]
            bg = small[:, 40:56]
            lru = small[:, 56:64]
            cvh = A.f32(8)
            cv1 = A.f32(8)
            bgh = A.f32(16)
            tmp8 = A.f32(8)
            act(tmp8, lru, AF.Exp, [Bsm], [Bsm], scale=-1.0)
            act(tmp8, tmp8, AF.Ln, [Bsm], [Bsm], bias=1.0)
            vts(cvh, tmp8, -4.0, None, ALU.mult, None, [Bsm], [Bsm])
            vts(cv1, tmp8, -8.0, None, ALU.mult, None, [Bsm], [Bsm])
            vts(bgh, bg, 0.5, None, ALU.mult, None, [Bsm], [Bsm])
            sets = [mkset(A, [("xbp0", "f", 515), ("xbp1", "f", 515), ("gb0", "f", 512), ("gb1", "f", 512),
                              ("xc0", "f", 512), ("xc1", "f", 512), ("xcb0", "b", 512), ("xcb1", "b", 512),
                              ("ri0", "f", 512), ("ri1", "f", 512), ("ri2", "f", 512), ("ri3", "f", 512),
                              ("yT0", "b", 512), ("yT1", "b", 512)]) for _ in range(2)]
            chs = [mkset(A, [("av", "f", 512), ("a2", "f", 512), ("bt", "f", 512), ("hs", "f", 512), ("g2", "f", 512)])] * 2
            hcar = A.f32(2)
            Bhc = Buf()
            wctx = {}

            def load_w(n):
                si = next_ws()
                wsv = wslot[si][:].rearrange("p (k n) -> p k n", k=8)
                wload(si, 0, wsv[:, :, 0:256], w_in[:, :, n * 256:(n + 1) * 256])
                wload(si, 1, wsv[:, :, 256:512], w_in[:, :, 1024 + n * 256:1024 + (n + 1) * 256])
                s2 = next_ws()
                wg = wslot[s2][:, 0:1024].rearrange("p (c e) -> p c e", c=2)
                wo = wslot[s2][:, 1024:3072].rearrange("p (c e) -> p c e", c=2)
                wload(s2, 0, wg, W["d_w_gates"][0, n].rearrange("(c p) e -> p c e", p=128))
                wload(s2, 1, wo, W["d_w_out"][0, n * 256:(n + 1) * 256, :].rearrange("(c p) e -> p c e", p=128))
                wctx[n] = (si, wsv, s2, wg, wo)

            def st0(it):
                n, tg = divmod(it, NG)
                if tg == 0:
                    load_w(n)
                si, wsv, s2, wg, wo = wctx[n]
                T = sets[it % 2]
                Tp = sets[(it + 1) % 2]
                for ch in range(2):
                    cc = n * 2 + ch
                    xbp, Bxbp = T["xbp%d" % ch], T["Bxbp%d" % ch]
                    gb, Bgb = T["gb%d" % ch], T["Bgb%d" % ch]
                    xc, Bxc = T["xc%d" % ch], T["Bxc%d" % ch]
                    xcb, Bxcb = T["xcb%d" % ch], T["Bxcb%d" % ch]
                    if tg == 0:
                        memset(xbp[:, 0:3], 0.0, [Bxbp])
                    else:
                        vcopy(xbp[:, 0:3], Tp["xbp%d" % ch][:, 512:515], [Tp["Bxbp%d" % ch]], [Bxbp])
                    px, Bpx = proj_fm(tg, wsv, 256 + ch * 128, Bws[si])
                    act(xbp[:, 3:515], px[:], AF.Copy, [Bpx], [Bxbp])
                    pg, Bpg = proj_fm(tg, wsv, ch * 128, Bws[si])
                    act(gb, pg[:], AF.Copy, [Bpg], [Bgb])
                    vts(xc, xbp[:, 0:512], cw[:, cc:cc + 1], cb[:, cc:cc + 1], ALU.mult, ALU.add,
                        [Bxbp, Bsm], [Bxc])
                    for j in range(1, 4):
                        vstt(xc, xbp[:, j:j + 512], cw[:, j * 8 + cc:j * 8 + cc + 1], xc,
                             ALU.mult, ALU.add, [Bxbp, Bsm, Bxc], [Bxc])
                    act(xcb, xc, AF.Copy, [Bxc], [Bxcb])

            def st1(it):
                n, tg = divmod(it, NG)
                si, wsv, s2, wg, wo = wctx[n]
                T = sets[it % 2]
                for ec in range(4):
                    pgt, Bpgt = next_psf()
                    for c2 in range(2):
                        mm(pgt[:], wg[:, c2, ec * 128:(ec + 1) * 128], T["xcb%d" % c2], c2 == 0, c2 == 1,
                           [Bws[s2], T["Bxcb%d" % c2]], [Bpgt])
                    act(T["ri%d" % ec], pgt[:], AF.Tanh, [Bpgt, Bsm], [T["Bri%d" % ec]],
                        bias=bgh[:, n * 4 + ec:n * 4 + ec + 1], scale=0.5)
                for ch in range(2):
                    cc = n * 2 + ch
                    C = chs[ch]
                    av, a2, bt, hs, g2 = C["av"], C["a2"], C["bt"], C["hs"], C["g2"]
                    Bav, Ba2, Bbt, Bhs, Bg2 = C["Bav"], C["Ba2"], C["Bbt"], C["Bhs"], C["Bg2"]
                    tr_, Btr = T["ri%d" % ch], T["Bri%d" % ch]
                    ti_, Bti = T["ri%d" % (2 + ch)], T["Bri%d" % (2 + ch)]
                    gb, Bgb = T["gb%d" % ch], T["Bgb%d" % ch]
                    xc, Bxc = T["xc%d" % ch], T["Bxc%d" % ch]
                    yT, ByT = T["yT%d" % ch], T["ByT%d" % ch]
                    act(av, tr_, AF.Exp, [Btr, Bsm], [Bav], scale=cvh[:, cc:cc + 1], bias=cvh[:, cc:cc + 1])
                    act(a2, tr_, AF.Exp, [Btr, Bsm], [Ba2], scale=cv1[:, cc:cc + 1], bias=cv1[:, cc:cc + 1])
                    act(a2, a2, AF.Sqrt, [Ba2], [Ba2], scale=-1.0, bias=1.0)
                    vstt(bt, ti_, 1.0, xc, ALU.add, ALU.mult, [Bti, Bxc], [Bbt])
                    vstt(bt, bt, 0.5, a2, ALU.mult, ALU.mult, [Bbt, Ba2], [Bbt])
                    init = 0.0 if tg == 0 else hcar[:, ch:ch + 1]
                    P.add("dve", lambda e, init=init, hs=hs, av=av, bt=bt: e.tensor_tensor_scan(
                        out=hs, data0=av, data1=bt, initial=init, op0=ALU.mult, op1=ALU.add),
                          reads=[Bav, Bbt, Bhc], writes=[Bhs], cost=1200.0)
                    vcopy(hcar[:, ch:ch + 1], hs[:, 511:512], [Bhs], [Bhc])
                    act(g2, gb, AF.Square, [Bgb], [Bg2])
                    vts(g2, g2, 0.044715, 1.0, ALU.mult, ALU.add, [Bg2], [Bg2])
                    vtt(g2, g2, gb, ALU.mult, [Bg2, Bgb], [Bg2])
                    act(g2, g2, AF.Tanh, [Bg2], [Bg2], scale=0.7978845608028654)
                    vstt(g2, g2, 1.0, gb, ALU.add, ALU.mult, [Bg2, Bgb], [Bg2])
                    vstt(yT, hs, 0.5, g2, ALU.mult, ALU.mult, [Bhs, Bg2], [ByT])
                for j in range(4):
                    t = tg * 4 + j
                    outproj_acc(t, [(T["yT%d" % c][:, j * 128:(j + 1) * 128], T["ByT%d" % c]) for c in range(2)],
                                lambda c, nh: wo[:, c, nh * 512:(nh + 1) * 512], Bws[s2])

            pipeline(4 * NG, [st0, st1])

        def mlstm_phase(l):
            A = Arena()
            w_in = W["a_w_in"][0].rearrange("(kc p) n -> p kc n", p=128)
            bgt = A.f32(8)
            gob = A.f32(1024)
            Bsm = Buf()
            dma("sp", bgt, W["a_b_gates"][0].partition_broadcast(128), [], [Bsm], next_io())
            dma("sp", gob, W["a_g_out"][0].rearrange("h v -> (h v)").partition_broadcast(128), [], [Bsm], next_io())
            gl = A.f32(NT * 8)
            gl3 = gl.rearrange("p (t g) -> p t g", g=8)
            Bgl = Buf()
            si = next_ws()
            wsv = wslot[si][:].rearrange("p (k n) -> p k n", k=8)
            wload(si, 0, wsv[:, :, 0:8], w_in[:, :, 3072:3080])
            for t in range(NT):
                ps, Bp = proj_tm(t, wsv, 0, 8, Bws[si])
                vtt(gl3[:, t, :], ps[:, 0:8], bgt, ALU.add, [Bp, Bsm], [Bgl])
            n4 = NT * 4
            xf = gl3[:, :, 4:8]
            xi = gl3[:, :, 0:4]
            t1 = A.f32(n4)
            t13 = t1.rearrange("p (t g) -> p t g", g=4)
            lf = A.f32(n4)
            lf3 = lf.rearrange("p (t g) -> p t g", g=4)
            bcs = A.f32(n4)
            bcs3 = bcs.rearrange("p (t g) -> p t g", g=4)
            ea = A.f32(n4)
            ea3 = ea.rearrange("p (t g) -> p t g", g=4)
            eb = A.f32(n4)
            eb3 = eb.rearrange("p (t g) -> p t g", g=4)
            ebt = A.f32(n4)
            ebt3 = ebt.rearrange("p (t g) -> p t g", g=4)
            act(t13, xf, AF.Abs, [Bgl], [Bgl])
            act(t1, t1, AF.Exp, [Bgl], [Bgl], scale=-1.0)
            act(t1, t1, AF.Ln, [Bgl], [Bgl], bias=1.0)
            vts(lf3, xf, 0.0, None, ALU.min, None, [Bgl], [Bgl])
            vtt(lf, lf, t1, ALU.subtract, [Bgl], [Bgl])
            pc, Bpc = next_psf()
            mm(pc[:, 0:n4], tri[:], lf, True, True, [Bconst, Bgl], [Bpc])
            vcopy(bcs, pc[:, 0:n4], [Bpc], [Bgl])
            pt, Bpt = next_psf()
            mm(pt[:, 0:n4], ones[:], lf, True, True, [Bconst, Bgl], [Bpt])
            act(ebt, pt[:, 0:n4], AF.Exp, [Bpt], [Bgl])
            act(eb, bcs, AF.Exp, [Bgl], [Bgl])
            vtt(ea3, xi, bcs3, ALU.subtract, [Bgl], [Bgl])
            act(ea, ea, AF.Exp, [Bgl], [Bgl])
            Cst = A.f32(257)
            Cbf = A.bf(258)
            BC, BCb = Buf(), Buf()
            NSET = 3
            sets = [mkset(A, [("qk", "b", 256), ("vaug", "b", 258), ("qkT", "b", 256), ("PT", "b", 128),
                              ("tmpC", "f", 257), ("sm", "f", 8), ("ht", "f", 256), ("og", "f", 256),
                              ("ybf", "b", 256), ("yT", "b", 256)]) for _ in range(NSET)]
            sc = 128.0 ** -0.25
            wctx = {}

            def load_w(hh):
                sa = next_ws()
                wa = wslot[sa][:].rearrange("p (k n) -> p k n", k=8)
                wload(sa, 0, wa[:, :, 0:128], w_in[:, :, hh * 128:(hh + 1) * 128])
                wload(sa, 0, wa[:, :, 128:256], w_in[:, :, 512 + hh * 128:512 + (hh + 1) * 128])
                wload(sa, 1, wa[:, :, 256:512], w_in[:, :, 1024 + hh * 256:1024 + (hh + 1) * 256])
                sb_ = next_ws()
                wb = wslot[sb_][:, 0:2048].rearrange("p (k n) -> p k n", k=8)
                wo = wslot[sb_][:, 2048:4096].rearrange("p (c e) -> p c e", c=2)
                wload(sb_, 0, wb, w_in[:, :, 2048 + hh * 256:2048 + (hh + 1) * 256])
                wload(sb_, 1, wo, W["a_w_out"][0, hh * 256:(hh + 1) * 256, :].rearrange("(c p) e -> p c e", p=128))
                wctx[hh] = (sa, wa, sb_, wb, wo)

            def st0(it):
                hh, c = divmod(it, NT)
                if c == 0:
                    load_w(hh)
                sa, wa, sb_, wb, wo = wctx[hh]
                T = sets[it % NSET]
                p1, Bp1 = proj_tm(c, wa, 0, 512, Bws[sa])
                p2, Bp2 = proj_tm(c, wb, 0, 256, Bws[sb_])
                act(T["qk"], p1[:, 0:256], AF.Copy, [Bp1], [T["Bqk"]], scale=sc)
                act(T["vaug"][:, 0:256], p1[:, 256:512], AF.Copy, [Bp1, Bgl], [T["Bvaug"]], scale=ea3[:, c, hh:hh + 1])
                vcopy(T["vaug"][:, 256:257], ea3[:, c, hh:hh + 1], [Bgl], [T["Bvaug"]])
                act(T["og"], p2[:, 0:256], AF.Sigmoid, [Bp2], [T["Bog"]])
                vtt(T["og"], T["og"], gob[:, hh * 256:(hh + 1) * 256], ALU.mult, [T["Bog"], Bsm], [T["Bog"]])

            def st1(it):
                hh, c = divmod(it, NT)
                sa, wa, sb_, wb, wo = wctx[hh]
                T = sets[it % NSET]
                qk, vaug, qkT, PT, tmpC, sm, ht, og, ybf, yT = [T[k] for k in
                    ("qk", "vaug", "qkT", "PT", "tmpC", "sm", "ht", "og", "ybf", "yT")]
                Bqk, Bva, BqkT, BPT, BtC, Bs, Bht, Bog, Bybf, ByT = [T["B" + k] for k in
                    ("qk", "vaug", "qkT", "PT", "tmpC", "sm", "ht", "og", "ybf", "yT")]
                if c == 0:
                    memset(Cst, 0.0, [BC])
                    memset(Cbf, 0.0, [BCb])
                pb, Bpb = next_psb()
                tr(pb[:, 0:128], qk[:, 0:128], [Bqk], [Bpb])
                tr(pb[:, 128:256], qk[:, 128:256], [Bqk], [Bpb])
                vcopy(qkT, pb[:, 0:256], [Bpb], [BqkT])
                pS, BpS = next_psf()
                mm(pS[:, 0:128], qkT[:, 128:256], qkT[:, 0:128], True, True, [BqkT], [BpS])
                vtt(PT, pS[:, 0:128], tri[:], ALU.mult, [BpS, Bconst], [BPT])
                po, Bpo = next_psf()
                mm(po[:, 0:257], PT, vaug[:, 0:257], True, False, [BPT, Bva], [Bpo])
                mm(po[:, 0:257], qkT[:, 0:128], Cbf[:, 0:257], False, True, [BqkT, BCb], [Bpo])
                pC, BpC = next_psf()
                mm(pC[:, 0:257], qk[:, 128:256], vaug[:, 0:257], True, True, [Bqk, Bva], [BpC])
                vtt(tmpC, pC[:, 0:257], Cst, ALU.add, [BpC, BC], [BtC])
                act(Cst, tmpC, AF.Copy, [BtC, Bgl], [BC], scale=ebt3[:, c, hh:hh + 1])
                act(Cbf[:, 0:257], tmpC, AF.Copy, [BtC, Bgl], [BCb], scale=ebt3[:, c, hh:hh + 1])
                ebc = eb3[:, c, hh:hh + 1]
                act(sm[:, 0:1], po[:, 256:257], AF.Abs, [Bpo, Bgl], [Bs], scale=ebc)
                vts(sm[:, 0:1], sm[:, 0:1], 1.0, None, ALU.max, None, [Bs], [Bs])
                vrecip(sm[:, 1:2], sm[:, 0:1], [Bs], [Bs])
                vtt(sm[:, 2:3], sm[:, 1:2], ebc, ALU.mult, [Bs, Bgl], [Bs])
                act(ht, po[:, 0:256], AF.Copy, [Bpo, Bs], [Bht], scale=sm[:, 2:3])
                act(scr[:, 0:256], ht, AF.Square, [Bht], [Bscr, Bs], accum_out=sm[:, 3:4])

            def st2(it):
                hh, c = divmod(it, NT)
                sa, wa, sb_, wb, wo = wctx[hh]
                T = sets[it % NSET]
                sm, ht, og, ybf, yT = [T[k] for k in ("sm", "ht", "og", "ybf", "yT")]
                Bs, Bht, Bog, Bybf, ByT = [T["B" + k] for k in ("sm", "ht", "og", "ybf", "yT")]
                act(sm[:, 4:5], sm[:, 3:4], AF.Sqrt, [Bs], [Bs], scale=1.0 / 256, bias=EPS)
                vrecip(sm[:, 5:6], sm[:, 4:5], [Bs], [Bs])
                vstt(ybf, ht, sm[:, 5:6], og, ALU.mult, ALU.mult, [Bht, Bs, Bog], [Bybf])
                pb2, Bpb2 = next_psb()
                tr(pb2[:, 0:128], ybf[:, 0:128], [Bybf], [Bpb2])
                tr(pb2[:, 128:256], ybf[:, 128:256], [Bybf], [Bpb2])
                vcopy(yT, pb2[:, 0:256], [Bpb2], [ByT])
                outproj_acc(c, [(yT[:, 0:128], ByT), (yT[:, 128:256], ByT)],
                            lambda cc, nh: wo[:, cc, nh * 512:(nh + 1) * 512], Bws[sb_])

            pipeline(4 * NT, [st0, st1, st2])

        def retention_phase(l):
            A = Arena()
            w_in = W["c_w_in"][0].rearrange("(kc p) n -> p kc n", p=128)
            cos_t = A.f32(NT * 128)
            sin_t = A.f32(NT * 128)
            Btab = Buf()
            mark = A.off
            trig_tables(A, invc[:], 128, cos_t, sin_t, Btab)
            A.off = mark
            cos3 = cos_t.rearrange("p (t i) -> p t i", i=128)
            sin3 = sin_t.rearrange("p (t i) -> p t i", i=128)
            P.fence()
            gobs = [A.f32(512) for _ in range(2)]
            Bgos = [Buf(), Buf()]
            R = [A.f32(512) for _ in range(2)]
            Rb = [A.bf(512) for _ in range(2)]
            BR = [Buf(), Buf()]
            BRb = [Buf(), Buf()]
            sets = [mkset(A, [("ra", "f", 256), ("rb", "f", 256), ("rc", "f", 256), ("rd", "f", 256), ("ro", "f", 512),
                              ("qkb", "b", 512), ("qkT", "b", 512), ("vb", "b", 512), ("PT", "b", 128),
                              ("tmpR0", "f", 512), ("tmpR1", "f", 512), ("sg", "f", 512), ("yn", "f", 512),
                              ("ybf", "b", 512), ("yT", "b", 512), ("st6", "f", 8)]) for _ in range(2)]
            wctx = {}

            def load_w(hh):
                gob, Bgo = gobs[hh % 2], Bgos[hh % 2]
                dma("sp", gob, W["c_g_out"][0, hh].partition_broadcast(128), [], [Bgo], next_io())
                sa = next_ws()
                wa = wslot[sa][:].rearrange("p (k n) -> p k n", k=8)
                wload(sa, 0, wa[:, :, 0:256], w_in[:, :, hh * 256:(hh + 1) * 256])
                wload(sa, 1, wa[:, :, 256:512], w_in[:, :, 1024 + hh * 256:1024 + (hh + 1) * 256])
                sv = next_ws()
                wv = wslot[sv][:].rearrange("p (k n) -> p k n", k=8)
                wload(sv, 0, wv, w_in[:, :, 2048 + hh * 512:2048 + (hh + 1) * 512])
                sg_ = next_ws()
                wgt = wslot[sg_][:].rearrange("p (k n) -> p k n", k=8)
                wload(sg_, 0, wgt, w_in[:, :, 4096 + hh * 512:4096 + (hh + 1) * 512])
                so = next_ws()
                wo = wslot[so][:].rearrange("p (c e) -> p c e", c=4)
                wload(so, 0, wo, W["c_w_out"][0, hh * 512:(hh + 1) * 512, :].rearrange("(c p) e -> p c e", p=128))
                wctx[hh] = (sa, wa, sv, wv, sg_, wgt, so, wo, gob, Bgo)

            def st0(it):
                hh, c = divmod(it, NT)
                sa, wa, sv, wv, sg_, wgt, so, wo, gob, Bgo = wctx[hh]
                T = sets[it % 2]
                ra, rb, rc, rd, vb, sg = [T[k] for k in ("ra", "rb", "rc", "rd", "vb", "sg")]
                Bra, Brb, Brc, Brd, Bvb, Bsg = [T["B" + k] for k in ("ra", "rb", "rc", "rd", "vb", "sg")]
                pqk, Bpqk = proj_tm(c, wa, 0, 512, Bws[sa])
                pv, Bpv = proj_tm(c, wv, 0, 512, Bws[sv])
                pg, Bpg = proj_tm(c, wgt, 0, 512, Bws[sg_])
                x4 = pqk[:].rearrange("p (q h i) -> p q h i", q=2, h=2)
                cb = bc_mid(cos3[:, c, :], 2)
                sb2 = bc_mid(sin3[:, c, :], 2)
                ra3 = ra.rearrange("p (q i) -> p q i", q=2)
                rb3 = rb.rearrange("p (q i) -> p q i", q=2)
                rc3 = rc.rearrange("p (q i) -> p q i", q=2)
                rd3 = rd.rearrange("p (q i) -> p q i", q=2)
                vtt(ra3, x4[:, :, 0, :], cb, ALU.mult, [Bpqk, Btab], [Bra])
                vtt(rb3, x4[:, :, 1, :], sb2, ALU.mult, [Bpqk, Btab], [Brb])
                vtt(rc3, x4[:, :, 1, :], cb, ALU.mult, [Bpqk, Btab], [Brc])
                vtt(rd3, x4[:, :, 0, :], sb2, ALU.mult, [Bpqk, Btab], [Brd])
                act(vb, pv[:], AF.Copy, [Bpv], [Bvb])
                act(sg, pg[:], AF.Silu, [Bpg], [Bsg])

            def st1(it):
                hh, c = divmod(it, NT)
                sa, wa, sv, wv, sg_, wgt, so, wo, gob, Bgo = wctx[hh]
                gch = RET_GCHUNK[hh]
                T = sets[it % 2]
                ra, rb, rc, rd, ro, qkb, qkT, vb, PT, sg, yn, ybf, yT, st6 = [T[k] for k in
                    ("ra", "rb", "rc", "rd", "ro", "qkb", "qkT", "vb", "PT", "sg", "yn", "ybf", "yT", "st6")]
                Bra, Brb, Brc, Brd, Bro, Bqkb, BqkT, Bvb, BPT, Bsg, Byn, Bybf, ByT, Bst = [T["B" + k] for k in
                    ("ra", "rb", "rc", "rd", "ro", "qkb", "qkT", "vb", "PT", "sg", "yn", "ybf", "yT", "st6")]
                if c == 0:
                    for d2 in range(2):
                        memset(R[d2], 0.0, [BR[d2]])
                        memset(Rb[d2], 0.0, [BRb[d2]])
                ra3 = ra.rearrange("p (q i) -> p q i", q=2)
                rb3 = rb.rearrange("p (q i) -> p q i", q=2)
                rc3 = rc.rearrange("p (q i) -> p q i", q=2)
                rd3 = rd.rearrange("p (q i) -> p q i", q=2)
                ro4 = ro.rearrange("p (q h i) -> p q h i", q=2, h=2)
                vtt(ro4[:, :, 0, :], ra3, rb3, ALU.subtract, [Bra, Brb], [Bro])
                vtt(ro4[:, :, 1, :], rc3, rd3, ALU.add, [Brc, Brd], [Bro])
                act(qkb[:, 0:256], ro[:, 0:256], AF.Copy, [Bro, Bconst], [Bqkb], scale=retc[:, hh:hh + 1])
                act(qkb[:, 256:512], ro[:, 256:512], AF.Copy, [Bro, Bconst], [Bqkb], scale=retc[:, 4 + hh:5 + hh])
                pb, Bpb = next_psb()
                for j in range(4):
                    tr(pb[:, j * 128:(j + 1) * 128], qkb[:, j * 128:(j + 1) * 128], [Bqkb], [Bpb])
                vcopy(qkT, pb[:, 0:512], [Bpb], [BqkT])
                pS, BpS = next_psf()
                for d2 in range(2):
                    mm(pS[:, 0:128], qkT[:, 256 + d2 * 128:256 + (d2 + 1) * 128], qkT[:, d2 * 128:(d2 + 1) * 128],
                       d2 == 0, d2 == 1, [BqkT], [BpS])
                vtt(PT, pS[:, 0:128], tri[:], ALU.mult, [BpS, Bconst], [BPT])
                py, Bpy = next_psf()
                mm(py[:], PT, vb, True, False, [BPT, Bvb], [Bpy])
                for d2 in range(2):
                    mm(py[:], qkT[:, d2 * 128:(d2 + 1) * 128], Rb[d2], False, d2 == 1, [BqkT, BRb[d2]], [Bpy])
                for d2 in range(2):
                    tmpR, BtR = T["tmpR%d" % d2], T["BtmpR%d" % d2]
                    pR, BpR = next_psf()
                    mm(pR[:], qkb[:, 256 + d2 * 128:256 + (d2 + 1) * 128], vb, True, True, [Bqkb, Bvb], [BpR])
                    vtt(tmpR, pR[:], R[d2], ALU.add, [BpR, BR[d2]], [BtR])
                    act(R[d2], tmpR, AF.Copy, [BtR], [BR[d2]], scale=gch)
                    act(Rb[d2], tmpR, AF.Copy, [BtR], [BRb[d2]], scale=gch)
                P.add("dve", lambda e, py=py, st6=st6: e.bn_stats(out=st6[:, 0:6], in_=py[:]), reads=[Bpy], writes=[Bst])
                P.add("dve", lambda e, st6=st6: e.bn_aggr(out=st6[:, 6:8], in_=st6[:, 0:6]), reads=[Bst], writes=[Bst])
                act(st6[:, 0:1], st6[:, 7:8], AF.Sqrt, [Bst], [Bst], bias=EPS)
                vrecip(st6[:, 1:2], st6[:, 0:1], [Bst], [Bst])
                vts(yn, py[:], st6[:, 6:7], st6[:, 1:2], ALU.subtract, ALU.mult, [Bpy, Bst], [Byn])
                vtt(sg, sg, gob, ALU.mult, [Bsg, Bgo], [Bsg])
                vtt(ybf, yn, sg, ALU.mult, [Byn, Bsg], [Bybf])
                pb2, Bpb2 = next_psb()
                for j in range(4):
                    tr(pb2[:, j * 128:(j + 1) * 128], ybf[:, j * 128:(j + 1) * 128], [Bybf], [Bpb2])
                vcopy(yT, pb2[:, 0:512], [Bpb2], [ByT])
                outproj_acc(c, [(yT[:, j * 128:(j + 1) * 128], ByT) for j in range(4)],
                            lambda cc, nh: wo[:, cc, nh * 512:(nh + 1) * 512], Bws[so])

            for hh in range(4):
                load_w(hh)
                pipeline(NT, [lambda c, hh=hh: st0(hh * NT + c), lambda c, hh=hh: st1(hh * NT + c)])

        def moba_phase(l):
            A = Arena()
            w_in = W["b_w_in"][0].rearrange("(kc p) n -> p kc n", p=128)
            cos_t = A.f32(NT * 16)
            sin_t = A.f32(NT * 16)
            Btab = Buf()
            mark = A.off
            trig_tables(A, invb[:], 16, cos_t, sin_t, Btab)
            A.off = mark
            cos3 = cos_t.rearrange("p (t i) -> p t i", i=16)
            sin3 = sin_t.rearrange("p (t i) -> p t i", i=16)
            P.fence()
            gqk = A.f32(256)
            Bg = Buf()
            dma("sp", gqk[:, 0:128], W["b_g_q"][0].partition_broadcast(128), [], [Bg], next_io())
            dma("sp", gqk[:, 128:256], W["b_g_k"][0].partition_broadcast(128), [], [Bg], next_io())
            NBLK = S // 256
            hsets = []
            for _ in range(2):
                d = {"qT": A.bf(S), "kT": A.bf(S), "vaug": A.bf(NT * 130), "kmf": A.f32(8), "kmb": A.bf(8)}
                d["BqT"] = [Buf() for _ in range(NT)]
                d["BkT"] = [Buf() for _ in range(NT)]
                d["Bva"] = [Buf() for _ in range(NT)]
                d["Bkm"] = Buf()
                hsets.append(d)
            NPSET = 3
            psets = [mkset(A, [("qkn", "f", 256), ("r4", "f", 128), ("qkb", "b", 256), ("sm", "f", 8)])
                     for _ in range(NPSET)]
            NQS = 4
            qsets = [mkset(A, [("gm", "f", 8), ("m8", "f", 8), ("sel", "f", 8), ("acc", "f", 132), ("obf", "b", 128),
                               ("yTt", "b", 128)]) for _ in range(NQS)]
            NPT = 4
            ptsets = [mkset(A, [("PT", "b", 256)]) for _ in range(NPT)]
            sc = 128.0 ** -0.5
            for hh in range(8):
                H = hsets[hh % 2]
                qT, kT, kmf, kmb = H["qT"], H["kT"], H["kmf"], H["kmb"]
                va3 = H["vaug"].rearrange("p (t e) -> p t e", e=130)
                BqT, BkT, Bva, Bkm = H["BqT"], H["BkT"], H["Bva"], H["Bkm"]
                sa = next_ws()
                wa = wslot[sa][:, 0:3072].rearrange("p (k n) -> p k n", k=8)
                wload(sa, 0, wa[:, :, 0:128], w_in[:, :, hh * 128:(hh + 1) * 128])
                wload(sa, 0, wa[:, :, 128:256], w_in[:, :, 1024 + hh * 128:1024 + (hh + 1) * 128])
                wload(sa, 1, wa[:, :, 256:384], w_in[:, :, 2048 + hh * 128:2048 + (hh + 1) * 128])
                wo = wslot[sa][:, 3072:4096]
                wload(sa, 1, wo, W["b_w_out"][0, hh * 128:(hh + 1) * 128, :])
                pctx = {}

                def ip0(c):
                    T = psets[c % NPSET]
                    qkn, sm = T["qkn"], T["sm"]
                    Bqkn, Bs = T["Bqkn"], T["Bsm"]
                    p1, Bp1 = proj_tm(c, wa, 0, 384, Bws[sa])
                    act(scr[:, 0:128], p1[:, 0:128], AF.Square, [Bp1], [Bscr, Bs], accum_out=sm[:, 0:1])
                    act(scr[:, 128:256], p1[:, 128:256], AF.Square, [Bp1], [Bscr, Bs], accum_out=sm[:, 1:2])
                    act(va3[:, c, 0:128], p1[:, 256:384], AF.Copy, [Bp1], [Bva[c]])
                    act(sm[:, 2:4], sm[:, 0:2], AF.Sqrt, [Bs], [Bs], scale=1.0 / 128, bias=EPS)
                    vrecip(sm[:, 4:6], sm[:, 2:4], [Bs], [Bs])
                    vstt(qkn[:, 0:128], p1[:, 0:128], sm[:, 4:5], gqk[:, 0:128], ALU.mult, ALU.mult,
                         [Bp1, Bs, Bg], [Bqkn])
                    vstt(qkn[:, 128:256], p1[:, 128:256], sm[:, 5:6], gqk[:, 128:256], ALU.mult, ALU.mult,
                         [Bp1, Bs, Bg], [Bqkn])
                    memset(va3[:, c, 128:129], 1.0, [Bva[c]])

                def ip1(c):
                    T = psets[c % NPSET]
                    qkn, r4, qkb = T["qkn"], T["r4"], T["qkb"]
                    Bqkn, Br4, Bqkb = T["Bqkn"], T["Br4"], T["Bqkb"]
                    q3 = qkn.rearrange("p (q d) -> p q d", q=2)
                    x1 = q3[:, :, 0:16]
                    x2 = q3[:, :, 16:32]
                    cb = bc_mid(cos3[:, c, :], 2)
                    sb2 = bc_mid(sin3[:, c, :], 2)
                    r43 = r4.rearrange("p (a q i) -> p a q i", a=4, q=2)
                    vtt(r43[:, 0], x1, cb, ALU.mult, [Bqkn, Btab], [Br4])
                    vtt(r43[:, 1], x2, sb2, ALU.mult, [Bqkn, Btab], [Br4])
                    vtt(r43[:, 2], x2, cb, ALU.mult, [Bqkn, Btab], [Br4])
                    vtt(r43[:, 3], x1, sb2, ALU.mult, [Bqkn, Btab], [Br4])
                    vtt(x1, r43[:, 0], r43[:, 1], ALU.subtract, [Br4], [Bqkn])
                    vtt(x2, r43[:, 2], r43[:, 3], ALU.add, [Br4], [Bqkn])
                    act(qkb[:, 0:128], qkn[:, 0:128], AF.Copy, [Bqkn], [Bqkb], scale=sc)
                    act(qkb[:, 128:256], qkn[:, 128:256], AF.Copy, [Bqkn], [Bqkb])
                    pb, Bpb = next_psb()
                    tr(pb[:, 0:128], qkb[:, 0:128], [Bqkb], [Bpb])
                    tr(pb[:, 128:256], qkb[:, 128:256], [Bqkb], [Bpb])
                    vcopy(qT[:, c * 128:(c + 1) * 128], pb[:, 0:128], [Bpb], [BqT[c]])
                    vcopy(kT[:, c * 128:(c + 1) * 128], pb[:, 128:256], [Bpb], [BkT[c]])

                pipeline(NT, [ip0, ip1])
                P.add("dve", lambda e, kmf=kmf, kT=kT: e.tensor_reduce(
                    out=kmf[:, 0:NBLK], in_=kT.rearrange("p (b n) -> p b n", n=256), axis=AX.X, op=ALU.add),
                      reads=BkT, writes=[Bkm], cost=2300.0)
                act(kmb[:, 0:NBLK], kmf[:, 0:NBLK], AF.Copy, [Bkm], [Bkm], scale=1.0 / 256)
                items = [(qi, b) for qi in range(NT) for b in range(qi // 2 + 1)]
                ictx = {}

                def at0(k):
                    qi, b = items[k]
                    j = qi // 2
                    own = (b == j)
                    chunks = [0] if (own and qi % 2 == 0) else [0, 1]
                    pS, BpS = next_psf()
                    for kc in chunks:
                        kt = b * 2 + kc
                        mm(pS[:, kc * 128:(kc + 1) * 128], kT[:, kt * 128:(kt + 1) * 128],
                           qT[:, qi * 128:(qi + 1) * 128], True, True, [BkT[kt], BqT[qi]], [BpS])
                    PTs = ptsets[k % NPT]
                    PT, BPT = PTs["PT"], PTs["BPT"]
                    n = len(chunks) * 128
                    act(PT[:, 0:n], pS[:, 0:n], AF.Exp, [BpS], [BPT])
                    if own:
                        dg = qi % 2
                        vtt(PT[:, dg * 128:(dg + 1) * 128], PT[:, dg * 128:(dg + 1) * 128], tribf[:], ALU.mult,
                            [BPT, Bconst], [BPT])
                    ictx[k] = chunks

                def at1(k):
                    qi, b = items[k]
                    j = qi // 2
                    nprev = j
                    own = (b == j)
                    chunks = ictx.pop(k)
                    Q = qsets[qi % NQS]
                    gm, m8, sel, acc, obf, yTt = Q["gm"], Q["m8"], Q["sel"], Q["acc"], Q["obf"], Q["yTt"]
                    Bgm, Bsel, Bacc, Bobf, ByT = Q["Bgm"], Q["Bsel"], Q["Bacc"], Q["Bobf"], Q["ByTt"]
                    PTs = ptsets[k % NPT]
                    PT, BPT = PTs["PT"], PTs["BPT"]
                    if b == 0 and nprev > 3:
                        pgt, Bpgt = next_psf()
                        mm(pgt[:, 0:NBLK], qT[:, qi * 128:(qi + 1) * 128], kmb[:, 0:NBLK], True, True,
                           [BqT[qi], Bkm], [Bpgt])
                        memset(gm, -1e30, [Bgm])
                        vcopy(gm[:, 0:nprev], pgt[:, 0:nprev], [Bpgt], [Bgm])
                        P.add("dve", lambda e, m8=m8, gm=gm: e.max(out=m8, in_=gm), reads=[Bgm], writes=[Bgm])
                        vts(sel, gm, m8[:, 2:3], None, ALU.is_ge, None, [Bgm], [Bsel])
                    pO, BpO = next_psf()
                    for ci, kc in enumerate(chunks):
                        kt = b * 2 + kc
                        mm(pO[:, 0:129], PT[:, kc * 128:(kc + 1) * 128], va3[:, kt, 0:129], ci == 0,
                           ci == len(chunks) - 1, [BPT, Bva[kt]], [BpO])
                    use_sel = (not own) and nprev > 3
                    if b == 0:
                        if use_sel:
                            vts(acc[:, 0:129], pO[:, 0:129], sel[:, b:b + 1], None, ALU.mult, None,
                                [BpO, Bsel], [Bacc])
                        else:
                            vcopy(acc[:, 0:129], pO[:, 0:129], [BpO], [Bacc])
                    else:
                        if use_sel:
                            vstt(acc[:, 0:129], pO[:, 0:129], sel[:, b:b + 1], acc[:, 0:129], ALU.mult, ALU.add,
                                 [BpO, Bsel, Bacc], [Bacc])
                        else:
                            vtt(acc[:, 0:129], pO[:, 0:129], acc[:, 0:129], ALU.add, [BpO, Bacc], [Bacc])
                    if own:
                        vrecip(acc[:, 130:131], acc[:, 128:129], [Bacc], [Bacc])
                        act(obf, acc[:, 0:128], AF.Copy, [Bacc], [Bobf], scale=acc[:, 130:131])

                def at2(k):
                    qi, b = items[k]
                    if b != qi // 2:
                        return
                    Q = qsets[qi % NQS]
                    obf, yTt, Bobf, ByT = Q["obf"], Q["yTt"], Q["Bobf"], Q["ByTt"]
                    pb2, Bpb2 = next_psb()
                    tr(pb2[:, 0:128], obf, [Bobf], [Bpb2])
                    vcopy(yTt, pb2[:, 0:128], [Bpb2], [ByT])
                    outproj_acc(qi, [(yTt, ByT)], lambda cc, nh: wo[:, nh * 512:(nh + 1) * 512], Bws[sa])

                def nop_stage(k):
                    pass

                pipeline(len(items), [at0, nop_stage, at1, nop_stage, at2])

        MIXERS = {0: mlstm_phase, 1: moba_phase, 2: retention_phase, 3: rglru_phase}

        _ARENA[0] = True
        for s in range(nseq):
            for t in range(NT):
                dma("sp", h[:, t, :], x_d[s, t * 128:(t + 1) * 128, :], [], [Bh[t]], next_io())
            dma("sp", posi[:], pos_d[s], [], [Bpos], next_io())
            vcopy(posf[:], posi[:], [Bpos], [Bpos])
            for kind, l in plan:
                if kind == "mix":
                    norm_phase(W["norm_mix"][l])
                    P.fence()
                    MIXERS[l](l)
                else:
                    norm_phase(W["norm_ffn"][l])
                    P.fence()
                    ffn_phase(l)
            outs = []
            for t in range(NT):
                outs.append(dma("sp", out_d[s, t * 128:(t + 1) * 128, :], h[:, t, :], [Bh[t]], [], next_io()))
            P.add("sp", None, extra_deps=[sl.last for sl in io_slots])
        _ARENA[0] = False
        P.finalize(sems)
        with nc.Block() as block:
            P.emit(block)
    return nc


def kernel(**inputs):
    ncores = 8
    x = np.ascontiguousarray(inputs["x"], dtype=np.float32)
    pos = np.asarray(inputs["positions"], dtype=np.int32)
    pos = np.ascontiguousarray(pos.reshape(pos.shape[0], -1, 128).transpose(0, 2, 1))
    B = x.shape[0]
    per = B // ncores
    nc = build(nseq=per, S=x.shape[1])
    consts = host_consts()
    in_maps = []
    for c in range(ncores):
        m = {"x": x[c * per:(c + 1) * per], "positions": pos[c * per:(c + 1) * per]}
        for k in DEV_PARAMS:
            m[k] = np.ascontiguousarray(inputs[k], dtype=np.float32)
        m["d_small"] = pack_d_small(inputs)
        m.update(consts)
        in_maps.append(m)
    res = run_bass_kernel_spmd(nc, in_maps, core_ids=list(range(ncores)))
    return np.concatenate([r["out"] for r in res.results], axis=0).astype(np.float32)
```

```python
import contextlib
import math
import numpy as np
import ml_dtypes
import concourse.bass as bass
import concourse.mybir as mybir
from concourse.bass_utils import run_bass_kernel_spmd

F32 = mybir.dt.float32
BF16 = mybir.dt.bfloat16
I32 = mybir.dt.int32
AF = mybir.ActivationFunctionType
ALU = mybir.AluOpType
AX = mybir.AxisListType

D = 1024
DFF = 2816
EPS = 1e-6
ENGS = ("pe", "act", "dve", "pool", "sp")


_ARENA = [False]


class Buf:
    __slots__ = ("name", "w", "r", "rd", "excl", "arena")

    def __init__(self, name="", excl=False):
        self.name = name
        self.arena = _ARENA[0]
        self.w = None
        self.r = []
        self.rd = []
        self.excl = excl


class Slot:
    __slots__ = ("sem", "count", "last")

    def __init__(self, sem):
        self.sem = sem
        self.count = 0
        self.last = None


class Op:
    __slots__ = ("eng", "fn", "deps", "pos", "sig", "sigidx", "waits", "slot", "slotval", "cost", "idx",
                 "succ", "nin", "ready", "fin", "barrier_slots", "tset", "deferred")

    def __init__(self, eng, fn):
        self.eng = eng
        self.fn = fn
        self.deps = {}
        self.pos = -1
        self.sig = False
        self.sigidx = 0
        self.waits = []
        self.slot = None
        self.slotval = 0
        self.cost = 100.0
        self.barrier_slots = None
        self.tset = None
        self.deferred = False


class Prog:
    def __init__(self):
        self.ops = {e: [] for e in ENGS}
        self.all = []
        self.slots = []
        self.epoch = Buf("arena_epoch")

    def fence(self):
        return self.add("sp", lambda e: e.nop(), writes=[self.epoch], cost=30.0)

    def add(self, eng, fn, reads=(), writes=(), slot=None, extra_deps=(), cost=100.0):
        op = Op(eng, fn)
        op.cost = cost
        deps = op.deps
        for b in reads:
            if b.arena:
                reads = list(reads) + [self.epoch]
                break
        else:
            for b in writes:
                if b.arena:
                    reads = list(reads) + [self.epoch]
                    break
        for b in reads:
            if b.w is not None:
                deps[b.w] = "raw"
            if b.excl:
                for r in b.r:
                    if r.eng != eng:
                        deps.setdefault(r, "war")
        for b in writes:
            if b.w is not None:
                deps.setdefault(b.w, "waw")
            for r in b.r:
                deps.setdefault(r, "war")
            for r in b.rd:
                deps.setdefault(r, "war")
        for d in extra_deps:
            if d is not None:
                deps[d] = "raw"
        if slot is not None:
            if slot.last is not None:
                deps[slot.last] = "raw"
            slot.count += 1
            op.slot = slot
            op.slotval = 16 * slot.count
            slot.last = op
        deps.pop(op, None)
        for b in writes:
            b.w = op
            b.r = []
            b.rd = []
        for b in reads:
            if slot is not None:
                b.rd.append(op)
            else:
                b.r.append(op)
        self.all.append(op)
        return op

    def barrier(self):
        b1 = Op("sp", lambda e: e.nop())
        b1.barrier_slots = [s.last for s in self.slots if s.last is not None]
        self.all.append(b1)
        return b1

    @staticmethod
    def _schedule(seg):
        import heapq
        inseg = set(seg)
        for i, op in enumerate(seg):
            op.idx = i
            op.succ = []
            op.nin = 0
            op.ready = 0.0
        for op in seg:
            for d in op.deps:
                if d in inseg:
                    d.succ.append(op)
                    op.nin += 1
        heap = [(0.0, op.idx, op) for op in seg if op.nin == 0]
        heapq.heapify(heap)
        free = {e: 0.0 for e in ENGS}
        order = []
        cur_set = -1
        while heap:
            rdy, _, op = heapq.heappop(heap)
            tl = 0.0
            if op.eng == "act" and op.tset is not None and cur_set not in op.tset:
                if not op.deferred:
                    op.deferred = True
                    heapq.heappush(heap, (max(rdy, free["act"]) + 1000.0, op.idx, op))
                    continue
                cur_set = min(op.tset)
                tl = 1300.0
            start = max(rdy, free[op.eng]) + tl
            if op.slot is not None:
                free[op.eng] = start + 120.0
                fin = start + op.cost
            else:
                fin = start + op.cost
                free[op.eng] = fin
            order.append(op)
            for s_ in op.succ:
                lat = 60.0 if (s_.eng == op.eng and op.slot is None) else 220.0
                t = fin + lat
                if t > s_.ready:
                    s_.ready = t
                s_.nin -= 1
                if s_.nin == 0:
                    heapq.heappush(heap, (s_.ready, s_.idx, s_))
        assert len(order) == len(seg)
        return order

    def finalize(self, sems, schedule=True):
        segs = [[]]
        bars = []
        for op in self.all:
            if op.barrier_slots is not None:
                bars.append(op)
                segs.append([])
            else:
                segs[-1].append(op)
        newall = []
        self.ops = {e: [] for e in ENGS}
        for si, seg in enumerate(segs):
            order = self._schedule(seg) if schedule else seg
            for op in order:
                op.pos = len(self.ops[op.eng])
                self.ops[op.eng].append(op)
                newall.append(op)
            if si < len(bars):
                b1 = bars[si]
                for e in ENGS:
                    for op in reversed(self.ops[e]):
                        if op.fn is not None:
                            b1.deps[op] = "raw"
                            break
                for d in b1.barrier_slots:
                    b1.deps[d] = "raw"
                b1.pos = len(self.ops["sp"])
                self.ops["sp"].append(b1)
                newall.append(b1)
                for e in ENGS:
                    if e != "sp":
                        w = Op(e, None)
                        w.deps[b1] = "raw"
                        w.pos = len(self.ops[e])
                        self.ops[e].append(w)
                        newall.append(w)
        self.all = newall
        known = {x: {y: -1 for y in ENGS} for x in ENGS}
        knownslot = {x: {} for x in ENGS}
        for op in self.all:
            x = op.eng
            for dep, kind in op.deps.items():
                if dep.slot is not None:
                    if knownslot[x].get(dep.slot, 0) >= dep.slotval:
                        continue
                    knownslot[x][dep.slot] = dep.slotval
                    op.waits.append((dep.slot.sem, dep.slotval))
                else:
                    y = dep.eng
                    if y == x and x == "pe":
                        assert dep.pos < op.pos
                        continue
                    if known[x][y] >= dep.pos:
                        continue
                    known[x][y] = dep.pos
                    dep.sig = True
                    op.waits.append((y, dep))
        for e in ENGS:
            c = 0
            for op in self.ops[e]:
                if op.sig:
                    c += 1
                    op.sigidx = c
        self.sems = sems

    def emit_engine(self, ename, eng):
        sems = self.sems
        for op in self.ops[ename]:
            for w in op.waits:
                if isinstance(w[0], str):
                    eng.wait_ge(sems[w[0]], w[1].sigidx)
                else:
                    eng.wait_ge(w[0], w[1])
            if op.fn is None:
                assert not op.sig
                continue
            inst = op.fn(eng)
            if op.slot is not None:
                inst.then_inc(op.slot.sem, 16)
            elif op.sig:
                inst.then_inc(sems[ename], 1)

    def emit(self, block):
        block.tensor(lambda e: self.emit_engine("pe", e))
        block.scalar(lambda e: self.emit_engine("act", e))
        block.vector(lambda e: self.emit_engine("dve", e))
        block.gpsimd(lambda e: self.emit_engine("pool", e))
        block.sync(lambda e: self.emit_engine("sp", e))


PARAM_SHAPES = {
    "norm_mix": [4, 1024], "norm_ffn": [4, 1024],
    "ffn_w_in": [4, 1024, 5632], "ffn_w_out": [4, 2816, 1024],
    "a_w_in": [1, 1024, 3080], "a_b_gates": [1, 8], "a_g_out": [1, 4, 256], "a_w_out": [1, 1024, 1024],
    "b_w_in": [1, 1024, 3072], "b_g_q": [1, 128], "b_g_k": [1, 128], "b_w_out": [1, 1024, 1024],
    "c_w_in": [1, 1024, 6144], "c_g_out": [1, 4, 512], "c_w_out": [1, 2048, 1024],
    "d_w_in": [1, 1024, 2048], "d_conv_w": [1, 4, 1024], "d_conv_b": [1, 1024],
    "d_w_gates": [1, 4, 256, 512], "d_b_gates": [1, 4, 512], "d_lru": [1, 1024], "d_w_out": [1, 1024, 1024],
}
DEV_PARAMS = [k for k in PARAM_SHAPES if k not in ("d_conv_w", "d_conv_b", "d_b_gates", "d_lru")]


def pack_d_small(inputs):
    cw = np.asarray(inputs["d_conv_w"], np.float32)[0].reshape(4, 8, 128).transpose(2, 0, 1).reshape(128, 32)
    cb = np.asarray(inputs["d_conv_b"], np.float32)[0].reshape(8, 128).T
    bg = np.asarray(inputs["d_b_gates"], np.float32)[0].reshape(4, 4, 128).transpose(2, 0, 1).reshape(128, 16)
    lr = np.asarray(inputs["d_lru"], np.float32)[0].reshape(8, 128).T
    return np.ascontiguousarray(np.concatenate([cw, cb, bg, lr], axis=1))


def host_consts():
    c = {}
    c["k_ident"] = np.eye(128, dtype=np.float32).astype(ml_dtypes.bfloat16)
    tri = (np.arange(128)[:, None] <= np.arange(128)[None, :]).astype(np.float32)
    c["k_tri"] = tri
    c["k_tribf"] = tri.astype(ml_dtypes.bfloat16)
    c["k_ones"] = np.ones((128, 128), np.float32)
    invb = (500000.0 ** (-np.arange(16, dtype=np.float32) * (2.0 / 32))).astype(np.float32)
    c["k_invb"] = np.broadcast_to(invb[None, :], (128, 16)).copy()
    invc = (10000.0 ** (-np.arange(128, dtype=np.float32) * (2.0 / 256))).astype(np.float32)
    c["k_invc"] = np.broadcast_to(invc[None, :], (128, 128)).copy()
    ret = np.zeros((128, 8), np.float64)
    p = np.arange(128, dtype=np.float64)
    for hh in range(4):
        lg = np.log1p(-np.exp2(-5.0 - hh))
        ret[:, hh] = np.exp((p + 1.0) * lg)
        ret[:, 4 + hh] = np.exp(-(p + 1.0) * lg) * (256 ** -0.5)
    c["k_ret"] = ret.astype(np.float32)
    return c


CONST_SPECS = {"k_ident": ([128, 128], BF16), "k_tri": ([128, 128], F32), "k_tribf": ([128, 128], BF16),
               "k_ones": ([128, 128], F32), "k_invb": ([128, 16], F32), "k_invc": ([128, 128], F32),
               "k_ret": ([128, 8], F32)}
RET_GCHUNK = [float(np.exp(128.0 * np.log1p(-np.exp2(-5.0 - hh)))) for hh in range(4)]


def build(nseq=2, S=2048, plan=None):
    if plan is None:
        plan = [("mix", l) if k == 0 else ("ffn", l) for l in range(4) for k in range(2)]
    NT = S // 128
    NG = S // 512
    nc = bass.Bass("TRN2", target_bir_lowering=False)
    x_d = nc.dram_tensor("x", [nseq, S, D], F32, kind="ExternalInput").ap()
    pos_d = nc.dram_tensor("positions", [nseq, 128, S // 128], I32, kind="ExternalInput").ap()
    W = {k: nc.dram_tensor(k, PARAM_SHAPES[k], F32, kind="ExternalInput").ap() for k in DEV_PARAMS}
    W["d_small"] = nc.dram_tensor("d_small", [128, 64], F32, kind="ExternalInput").ap()
    KC = {k: nc.dram_tensor(k, v[0], v[1], kind="ExternalInput").ap() for k, v in CONST_SPECS.items()}
    out_d = nc.dram_tensor("out", [nseq, S, D], F32, kind="ExternalOutput").ap()

    P = Prog()
    with contextlib.ExitStack() as st:
        def sb(name, shape, dt):
            return st.enter_context(nc.sbuf_tensor(name, shape, dt))

        def pst(name, shape, dt):
            return st.enter_context(nc.psum_tensor(name, shape, dt))

        def newsem(name):
            return st.enter_context(nc.semaphore(name))

        sems = {e: newsem("s_" + e) for e in ENGS}
        nslots = [0]

        def newslot():
            s = Slot(newsem("d%d" % nslots[0]))
            nslots[0] += 1
            P.slots.append(s)
            return s

        h = sb("h", [128, NT, D], F32)
        Bh = [Buf("h%d" % t) for t in range(NT)]
        uT = sb("uT", [128, 8, S], BF16)
        BuT = [Buf("uT%d" % t) for t in range(NT)]
        NWS = 4
        wslot = [sb("ws%d" % i, [128, 4096], BF16) for i in range(NWS)]
        Bws = [Buf("ws%d" % i) for i in range(NWS)]
        ws_dslots = [(newslot(), newslot()) for _ in range(NWS)]
        ws_rr = [0]
        gbc = sb("gbc", [128, D], F32)
        Bgbc = Buf("gbc")
        gbc_slot = newslot()
        ARENA = 16640
        arena = sb("arena", [128, ARENA], F32)
        arena_bf = arena[:].bitcast(BF16)
        scr = sb("scr", [128, D], BF16)
        Bscr = Buf("scr")
        ubf = [sb("ubf%d" % i, [128, D], BF16) for i in range(2)]
        Bubf = [Buf("ubf%d" % i) for i in range(2)]
        stat = sb("stat", [128, 3 * NT], F32)
        Bstat = [Buf("stat%d" % t) for t in range(NT)]
        ident = sb("ident", [128, 128], BF16)
        tri = sb("tri", [128, 128], F32)
        tribf = sb("tribf", [128, 128], BF16)
        ones = sb("ones", [128, 128], F32)
        invb = sb("invb", [128, 16], F32)
        invc = sb("invc", [128, 128], F32)
        retc = sb("retc", [128, 8], F32)
        neghalf = sb("neghalf", [128, 2], F32)
        Bconst = Buf("const")
        posi = sb("posi", [128, NT], I32)
        posf = sb("posf", [128, NT], F32)
        Bpos = Buf("pos")

        NPS = 6
        psf = [pst("psf%d" % i, [128, 512], F32) for i in range(NPS)]
        Bpsf = [Buf("psf%d" % i, excl=True) for i in range(NPS)]
        psb = [pst("psb%d" % i, [128, 1024], BF16) for i in range(2)]
        Bpsb = [Buf("psb%d" % i, excl=True) for i in range(2)]
        rr = {"psf": 0, "psb": 0, "io": 0, "ubf": 0}

        def next_psf():
            i = rr["psf"]
            rr["psf"] = (i + 1) % NPS
            return psf[i], Bpsf[i]

        def next_psb():
            i = rr["psb"]
            rr["psb"] = (i + 1) % 2
            return psb[i], Bpsb[i]

        io_slots = [newslot() for _ in range(4)]

        def next_io():
            i = rr["io"]
            rr["io"] = (i + 1) % 4
            return io_slots[i]

        def next_ws():
            i = ws_rr[0]
            ws_rr[0] = (i + 1) % NWS
            return i

        ACT_SETS = {AF.Exp: (0, 6), AF.Tanh: (0, 2), AF.Sigmoid: (2,), AF.Sqrt: (3,), AF.Ln: (5, 6),
                    AF.Silu: (20,), AF.Sin: (21,)}

        def fsz(ap):
            n = 1
            for d in ap.shape[1:]:
                n *= int(d)
            return n

        def dma(eng, out, in_, reads, writes, slot):
            nbytes = fsz(out) * 128 * 4
            return P.add(eng, lambda e: e.dma_start(out=out, in_=in_), reads=reads, writes=writes, slot=slot,
                         cost=2500.0 + nbytes / 150.0)

        def wload(i, part, out, in_):
            return dma("pool", out, in_, [], [Bws[i]], ws_dslots[i][part])

        def mm(out, lhsT, rhs, start, stop, reads, writes):
            fp32 = (lhsT.dtype == F32)
            return P.add("pe", lambda e: e.matmul(out, lhsT=lhsT, rhs=rhs, start=start, stop=stop),
                         reads=reads, writes=writes, cost=max(60.0, fsz(out) * (0.5 if not fp32 else 2.0)) + 10.0)

        def tr(out, in_, reads, writes):
            return P.add("pe", lambda e: e.transpose(out=out, in_=in_, identity=ident[:]),
                         reads=list(reads) + [Bconst], writes=writes, cost=65.0)

        def act(out, in_, func, reads, writes, **kw):
            op = P.add("act", lambda e: e.activation(out=out, in_=in_, func=func, **kw), reads=reads, writes=writes,
                       cost=230.0 + fsz(out) * 0.85 + (100.0 if "accum_out" in kw else 0.0))
            op.tset = ACT_SETS.get(func)
            return op

        def vtt(out, in0, in1, op, reads, writes, eng="dve"):
            return P.add(eng, lambda e: e.tensor_tensor(out=out, in0=in0, in1=in1, op=op), reads=reads, writes=writes,
                         cost=70.0 + fsz(out) * 1.2)

        def vts(out, in0, s1, s2, op0, op1, reads, writes, eng="dve"):
            if op1 is None:
                return P.add(eng, lambda e: e.tensor_scalar(out=out, in0=in0, scalar1=s1, scalar2=None, op0=op0),
                             reads=reads, writes=writes, cost=70.0 + fsz(out) * 0.7)
            return P.add(eng, lambda e: e.tensor_scalar(out=out, in0=in0, scalar1=s1, scalar2=s2, op0=op0, op1=op1),
                         reads=reads, writes=writes, cost=70.0 + fsz(out) * 0.7)

        def vstt(out, in0, scalar, in1, op0, op1, reads, writes):
            return P.add("dve", lambda e: e.scalar_tensor_tensor(out=out, in0=in0, scalar=scalar, in1=in1,
                                                                 op0=op0, op1=op1), reads=reads, writes=writes,
                         cost=70.0 + fsz(out) * 1.1)

        def vcopy(out, in_, reads, writes, eng="dve"):
            return P.add(eng, lambda e: e.tensor_copy(out=out, in_=in_), reads=reads, writes=writes,
                         cost=70.0 + fsz(out) * 1.0)

        def vrecip(out, in_, reads, writes):
            return P.add("dve", lambda e: e.reciprocal(out=out, in_=in_), reads=reads, writes=writes,
                         cost=70.0 + fsz(out) * 8.0)

        def memset(ap, val, writes, eng="dve"):
            return P.add(eng, lambda e: e.memset(ap, val), writes=writes)

        for name, t in (("k_ident", ident), ("k_tri", tri), ("k_tribf", tribf), ("k_ones", ones),
                        ("k_invb", invb), ("k_invc", invc), ("k_ret", retc)):
            dma("sp", t[:], KC[name], [], [Bconst], next_io())


        def norm_phase(g_row):
            dma("sp", gbc[:], g_row.partition_broadcast(128), [], [Bgbc], gbc_slot)
            for t in range(NT):
                ss = stat[:, 3 * t:3 * t + 1]
                sq = stat[:, 3 * t + 1:3 * t + 2]
                rs = stat[:, 3 * t + 2:3 * t + 3]
                act(scr[:], h[:, t, :], AF.Square, [Bh[t]], [Bscr, Bstat[t]], accum_out=ss)
                act(sq, ss, AF.Sqrt, [Bstat[t]], [Bstat[t]], scale=1.0 / D, bias=EPS)
                vrecip(rs, sq, [Bstat[t]], [Bstat[t]])
                ui = rr["ubf"]
                rr["ubf"] = 1 - ui
                vstt(ubf[ui][:], h[:, t, :], rs, gbc[:], ALU.mult, ALU.mult, [Bh[t], Bstat[t], Bgbc], [Bubf[ui]])
                pb, Bpb = next_psb()
                for k in range(8):
                    tr(pb[:, k * 128:(k + 1) * 128], ubf[ui][:, k * 128:(k + 1) * 128], [Bubf[ui]], [Bpb])
                vcopy(uT[:, :, t * 128:(t + 1) * 128], pb[:].rearrange("p (k c) -> p k c", k=8), [Bpb], [BuT[t]])

        def ffn_phase(l):
            w_in = W["ffn_w_in"][l].rearrange("(kc p) n -> p kc n", p=128)
            w_out = W["ffn_w_out"][l]
            hT = arena_bf[:, 0:6 * S].rearrange("p (c s) -> p c s", c=6)
            BhT = [[Buf() for _ in range(NG)] for _ in range(6)]
            wo = [arena_bf[:, 6 * S + i * 6144: 6 * S + (i + 1) * 6144].rearrange("p (c n) -> p c n", c=6)
                  for i in range(2)]
            Bwo = [Buf(), Buf()]
            wo_slot = [newslot(), newslot()]
            sg = [arena[:, ARENA - 1024 + i * 512: ARENA - 1024 + (i + 1) * 512] for i in range(2)]
            assert 6 * S + 2 * 6144 <= 2 * ARENA - 2048
            Bsg = [Buf(), Buf()]
            sgi = 0
            quarters = [[0, 1, 2], [3, 4, 5], [6, 7, 8], [9, 10]]
            for qi, groups in enumerate(quarters):
                nch = 2 * len(groups)
                r0 = groups[0] * 256
                wi = qi % 2
                dma("pool", wo[wi][:, 0:nch, :], w_out[r0:r0 + nch * 128, :].rearrange("(c p) n -> p c n", p=128),
                    [], [Bwo[wi]], wo_slot[wi])
                for gi, g in enumerate(groups):
                    si = next_ws()
                    wsv = wslot[si][:].rearrange("p (k n) -> p k n", k=8)
                    wload(si, 0, wsv[:, :, 0:256], w_in[:, :, g * 256:(g + 1) * 256])
                    wload(si, 1, wsv[:, :, 256:512], w_in[:, :, DFF + g * 256:DFF + (g + 1) * 256])
                    for half in range(2):
                        ch = gi * 2 + half
                        for tg in range(NG):
                            pg, Bpg = next_psf()
                            pu, Bpu = next_psf()
                            ur = [BuT[tg * 4 + i] for i in range(4)]
                            for kc in range(8):
                                mm(pg[:], wsv[:, kc, half * 128:(half + 1) * 128], uT[:, kc, tg * 512:(tg + 1) * 512],
                                   kc == 0, kc == 7, [Bws[si]] + ur, [Bpg])
                            for kc in range(8):
                                mm(pu[:], wsv[:, kc, 256 + half * 128:256 + (half + 1) * 128],
                                   uT[:, kc, tg * 512:(tg + 1) * 512], kc == 0, kc == 7, [Bws[si]] + ur, [Bpu])
                            act(sg[sgi][:], pg[:], AF.Silu, [Bpg], [Bsg[sgi]])
                            vtt(hT[:, ch, tg * 512:(tg + 1) * 512], pu[:], sg[sgi][:], ALU.mult,
                                [Bpu, Bsg[sgi]], [BhT[ch][tg]])
                            sgi = 1 - sgi
                for t in range(NT):
                    for nh in range(2):
                        po, Bpo = next_psf()
                        for c in range(nch):
                            mm(po[:], hT[:, c, t * 128:(t + 1) * 128], wo[wi][:, c, nh * 512:(nh + 1) * 512],
                               c == 0, c == nch - 1, [BhT[c][t // 4], Bwo[wi]], [Bpo])
                        vtt(h[:, t, nh * 512:(nh + 1) * 512], po[:], h[:, t, nh * 512:(nh + 1) * 512], ALU.add,
                            [Bpo, Bh[t]], [Bh[t]])

        class Arena:
            def __init__(self):
                self.off = 0

            def f32(self, n):
                o = self.off
                self.off += n
                assert self.off <= ARENA, self.off
                return arena[:, o:o + n]

            def bf(self, n):
                o = self.off
                self.off += (n + 1) // 2
                assert self.off <= ARENA, self.off
                return arena_bf[:, 2 * o:2 * o + n]

            def i32(self, n):
                return self.f32(n).bitcast(I32)

        def mkset(A, spec):
            d = {}
            for name, kind, n in spec:
                d[name] = A.f32(n) if kind == "f" else A.bf(n)
                d["B" + name] = Buf()
            return d

        def bc_mid(ap2d, n):
            return ap2d.unsqueeze(1).broadcast_to([ap2d.shape[0], n, ap2d.shape[1]])

        def proj_tm(t, wsv, c0, n, Bw):
            ps, Bp = next_psf()
            for kc in range(8):
                mm(ps[:, 0:n], uT[:, kc, t * 128:(t + 1) * 128], wsv[:, kc, c0:c0 + n], kc == 0, kc == 7,
                   [BuT[t], Bw], [Bp])
            return ps, Bp

        def proj_fm(tg, wsv, c0, Bw):
            ps, Bp = next_psf()
            ur = [BuT[tg * 4 + i] for i in range(4)]
            for kc in range(8):
                mm(ps[:], wsv[:, kc, c0:c0 + 128], uT[:, kc, tg * 512:(tg + 1) * 512], kc == 0, kc == 7,
                   [Bw] + ur, [Bp])
            return ps, Bp

        def outproj_acc(t, lhs_list, wfn, Bw):
            n = len(lhs_list)
            for nh in range(2):
                po, Bpo = next_psf()
                for c, (lap, lb) in enumerate(lhs_list):
                    mm(po[:], lap, wfn(c, nh), c == 0, c == n - 1, [lb, Bw], [Bpo])
                vtt(h[:, t, nh * 512:(nh + 1) * 512], po[:], h[:, t, nh * 512:(nh + 1) * 512], ALU.add,
                    [Bpo, Bh[t]], [Bh[t]])

        def trig_tables(A, inv_tile, ni, cos_out, sin_out, Bt):
            n = NT * ni
            ang = A.f32(n)
            angs = A.f32(n)
            kf = A.f32(n)
            ki = A.i32(n)
            Bl = Buf()
            ang3 = ang.rearrange("p (t i) -> p t i", i=ni)
            for t in range(NT):
                vts(ang3[:, t, :], inv_tile, posf[:, t:t + 1], None, ALU.mult, None, [Bconst, Bpos], [Bl])
            PI = math.pi
            for shift, out in ((0.0, sin_out), (PI / 2, cos_out)):
                vts(angs, ang, shift, None, ALU.add, None, [Bl], [Bl])
                vts(kf, angs, 1.0 / (2 * PI), None, ALU.mult, None, [Bl], [Bl])
                vcopy(ki, kf, [Bl], [Bl])
                vcopy(kf, ki, [Bl], [Bl])
                vstt(angs, kf, -2 * PI, angs, ALU.mult, ALU.add, [Bl], [Bl])
                vts(angs, angs, -3.14159, 3.14159, ALU.max, ALU.min, [Bl], [Bl])
                act(out, angs, AF.Sin, [Bl], [Bt, Bl])

        def pipeline(n, stages):
            ns = len(stages)
            for k in range(n + ns - 1):
                for s_, f in enumerate(stages):
                    i = k - s_
                    if 0 <= i < n:
                        f(i)

        def rglru_phase(l):
            A = Arena()
            w_in = W["d_w_in"][0].rearrange("(kc p) n -> p kc n", p=128)
            small = A.f32(64)
            Bsm = Buf()
            dma("sp", small, W["d_small"], [], [Bsm], next_io())
            cw = small[:, 0:32]
            cb = small[:, 32:40]
            bg = small[:, 40:56]
            lru = small[:, 56:64]
            cvh = A.f32(8)
            cv1 = A.f32(8)
            bgh = A.f32(16)
            tmp8 = A.f32(8)
            act(tmp8, lru, AF.Exp, [Bsm], [Bsm], scale=-1.0)
            act(tmp8, tmp8, AF.Ln, [Bsm], [Bsm], bias=1.0)
            vts(cvh, tmp8, -4.0, None, ALU.mult, None, [Bsm], [Bsm])
            vts(cv1, tmp8, -8.0, None, ALU.mult, None, [Bsm], [Bsm])
            vts(bgh, bg, 0.5, None, ALU.mult, None, [Bsm], [Bsm])
            sets = [mkset(A, [("xbp0", "f", 515), ("xbp1", "f", 515), ("gb0", "f", 512), ("gb1", "f", 512),
                              ("xc0", "f", 512), ("xc1", "f", 512), ("xcb0", "b", 512), ("xcb1", "b", 512),
                              ("ri0", "f", 512), ("ri1", "f", 512), ("ri2", "f", 512), ("ri3", "f", 512),
                              ("yT0", "b", 512), ("yT1", "b", 512)]) for _ in range(2)]
            chs = [mkset(A, [("av", "f", 512), ("a2", "f", 512), ("bt", "f", 512), ("hs", "f", 512), ("g2", "f", 512)])] * 2
            hcar = A.f32(2)
            Bhc = Buf()
            wctx = {}

            def load_w(n):
                si = next_ws()
                wsv = wslot[si][:].rearrange("p (k n) -> p k n", k=8)
                wload(si, 0, wsv[:, :, 0:256], w_in[:, :, n * 256:(n + 1) * 256])
                wload(si, 1, wsv[:, :, 256:512], w_in[:, :, 1024 + n * 256:1024 + (n + 1) * 256])
                s2 = next_ws()
                wg = wslot[s2][:, 0:1024].rearrange("p (c e) -> p c e", c=2)
                wo = wslot[s2][:, 1024:3072].rearrange("p (c e) -> p c e", c=2)
                wload(s2, 0, wg, W["d_w_gates"][0, n].rearrange("(c p) e -> p c e", p=128))
                wload(s2, 1, wo, W["d_w_out"][0, n * 256:(n + 1) * 256, :].rearrange("(c p) e -> p c e", p=128))
                wctx[n] = (si, wsv, s2, wg, wo)

            def st0(it):
                n, tg = divmod(it, NG)
                if tg == 0:
                    load_w(n)
                si, wsv, s2, wg, wo = wctx[n]
                T = sets[it % 2]
                Tp = sets[(it + 1) % 2]
                for ch in range(2):
                    cc = n * 2 + ch
                    xbp, Bxbp = T["xbp%d" % ch], T["Bxbp%d" % ch]
                    gb, Bgb = T["gb%d" % ch], T["Bgb%d" % ch]
                    xc, Bxc = T["xc%d" % ch], T["Bxc%d" % ch]
                    xcb, Bxcb = T["xcb%d" % ch], T["Bxcb%d" % ch]
                    if tg == 0:
                        memset(xbp[:, 0:3], 0.0, [Bxbp])
                    else:
                        vcopy(xbp[:, 0:3], Tp["xbp%d" % ch][:, 512:515], [Tp["Bxbp%d" % ch]], [Bxbp])
                    px, Bpx = proj_fm(tg, wsv, 256 + ch * 128, Bws[si])
                    act(xbp[:, 3:515], px[:], AF.Copy, [Bpx], [Bxbp])
                    pg, Bpg = proj_fm(tg, wsv, ch * 128, Bws[si])
                    act(gb, pg[:], AF.Copy, [Bpg], [Bgb])
                    vts(xc, xbp[:, 0:512], cw[:, cc:cc + 1], cb[:, cc:cc + 1], ALU.mult, ALU.add,
                        [Bxbp, Bsm], [Bxc])
                    for j in range(1, 4):
                        vstt(xc, xbp[:, j:j + 512], cw[:, j * 8 + cc:j * 8 + cc + 1], xc,
                             ALU.mult, ALU.add, [Bxbp, Bsm, Bxc], [Bxc])
                    act(xcb, xc, AF.Copy, [Bxc], [Bxcb])

            def st1(it):
                n, tg = divmod(it, NG)
                si, wsv, s2, wg, wo = wctx[n]
                T = sets[it % 2]
                for ec in range(4):
                    pgt, Bpgt = next_psf()
                    for c2 in range(2):
                        mm(pgt[:], wg[:, c2, ec * 128:(ec + 1) * 128], T["xcb%d" % c2], c2 == 0, c2 == 1,
                           [Bws[s2], T["Bxcb%d" % c2]], [Bpgt])
                    act(T["ri%d" % ec], pgt[:], AF.Tanh, [Bpgt, Bsm], [T["Bri%d" % ec]],
                        bias=bgh[:, n * 4 + ec:n * 4 + ec + 1], scale=0.5)
                for ch in range(2):
                    cc = n * 2 + ch
                    C = chs[ch]
                    av, a2, bt, hs, g2 = C["av"], C["a2"], C["bt"], C["hs"], C["g2"]
                    Bav, Ba2, Bbt, Bhs, Bg2 = C["Bav"], C["Ba2"], C["Bbt"], C["Bhs"], C["Bg2"]
                    tr_, Btr = T["ri%d" % ch], T["Bri%d" % ch]
                    ti_, Bti = T["ri%d" % (2 + ch)], T["Bri%d" % (2 + ch)]
                    gb, Bgb = T["gb%d" % ch], T["Bgb%d" % ch]
                    xc, Bxc = T["xc%d" % ch], T["Bxc%d" % ch]
                    yT, ByT = T["yT%d" % ch], T["ByT%d" % ch]
                    act(av, tr_, AF.Exp, [Btr, Bsm], [Bav], scale=cvh[:, cc:cc + 1], bias=cvh[:, cc:cc + 1])
                    act(a2, tr_, AF.Exp, [Btr, Bsm], [Ba2], scale=cv1[:, cc:cc + 1], bias=cv1[:, cc:cc + 1])
                    act(a2, a2, AF.Sqrt, [Ba2], [Ba2], scale=-1.0, bias=1.0)
                    vstt(bt, ti_, 1.0, xc, ALU.add, ALU.mult, [Bti, Bxc], [Bbt])
                    vstt(bt, bt, 0.5, a2, ALU.mult, ALU.mult, [Bbt, Ba2], [Bbt])
                    init = 0.0 if tg == 0 else hcar[:, ch:ch + 1]
                    P.add("dve", lambda e, init=init, hs=hs, av=av, bt=bt: e.tensor_tensor_scan(
                        out=hs, data0=av, data1=bt, initial=init, op0=ALU.mult, op1=ALU.add),
                          reads=[Bav, Bbt, Bhc], writes=[Bhs], cost=1200.0)
                    vcopy(hcar[:, ch:ch + 1], hs[:, 511:512], [Bhs], [Bhc])
                    act(g2, gb, AF.Square, [Bgb], [Bg2])
                    vts(g2, g2, 0.044715, 1.0, ALU.mult, ALU.add, [Bg2], [Bg2])
                    vtt(g2, g2, gb, ALU.mult, [Bg2, Bgb], [Bg2])
                    act(g2, g2, AF.Tanh, [Bg2], [Bg2], scale=0.7978845608028654)
                    vstt(g2, g2, 1.0, gb, ALU.add, ALU.mult, [Bg2, Bgb], [Bg2])
                    vstt(yT, hs, 0.5, g2, ALU.mult, ALU.mult, [Bhs, Bg2], [ByT])
                for j in range(4):
                    t = tg * 4 + j
                    outproj_acc(t, [(T["yT%d" % c][:, j * 128:(j + 1) * 128], T["ByT%d" % c]) for c in range(2)],
                                lambda c, nh: wo[:, c, nh * 512:(nh + 1) * 512], Bws[s2])

            pipeline(4 * NG, [st0, st1])

        def mlstm_phase(l):
            A = Arena()
            w_in = W["a_w_in"][0].rearrange("(kc p) n -> p kc n", p=128)
            bgt = A.f32(8)
            gob = A.f32(1024)
            Bsm = Buf()
            dma("sp", bgt, W["a_b_gates"][0].partition_broadcast(128), [], [Bsm], next_io())
            dma("sp", gob, W["a_g_out"][0].rearrange("h v -> (h v)").partition_broadcast(128), [], [Bsm], next_io())
            gl = A.f32(NT * 8)
            gl3 = gl.rearrange("p (t g) -> p t g", g=8)
            Bgl = Buf()
            si = next_ws()
            wsv = wslot[si][:].rearrange("p (k n) -> p k n", k=8)
            wload(si, 0, wsv[:, :, 0:8], w_in[:, :, 3072:3080])
            for t in range(NT):
                ps, Bp = proj_tm(t, wsv, 0, 8, Bws[si])
                vtt(gl3[:, t, :], ps[:, 0:8], bgt, ALU.add, [Bp, Bsm], [Bgl])
            n4 = NT * 4
            xf = gl3[:, :, 4:8]
            xi = gl3[:, :, 0:4]
            t1 = A.f32(n4)
            t13 = t1.rearrange("p (t g) -> p t g", g=4)
            lf = A.f32(n4)
            lf3 = lf.rearrange("p (t g) -> p t g", g=4)
            bcs = A.f32(n4)
            bcs3 = bcs.rearrange("p (t g) -> p t g", g=4)
            ea = A.f32(n4)
            ea3 = ea.rearrange("p (t g) -> p t g", g=4)
            eb = A.f32(n4)
            eb3 = eb.rearrange("p (t g) -> p t g", g=4)
            ebt = A.f32(n4)
            ebt3 = ebt.rearrange("p (t g) -> p t g", g=4)
            act(t13, xf, AF.Abs, [Bgl], [Bgl])
            act(t1, t1, AF.Exp, [Bgl], [Bgl], scale=-1.0)
            act(t1, t1, AF.Ln, [Bgl], [Bgl], bias=1.0)
            vts(lf3, xf, 0.0, None, ALU.min, None, [Bgl], [Bgl])
            vtt(lf, lf, t1, ALU.subtract, [Bgl], [Bgl])
            pc, Bpc = next_psf()
            mm(pc[:, 0:n4], tri[:], lf, True, True, [Bconst, Bgl], [Bpc])
            vcopy(bcs, pc[:, 0:n4], [Bpc], [Bgl])
            pt, Bpt = next_psf()
            mm(pt[:, 0:n4], ones[:], lf, True, True, [Bconst, Bgl], [Bpt])
            act(ebt, pt[:, 0:n4], AF.Exp, [Bpt], [Bgl])
            act(eb, bcs, AF.Exp, [Bgl], [Bgl])
            vtt(ea3, xi, bcs3, ALU.subtract, [Bgl], [Bgl])
            act(ea, ea, AF.Exp, [Bgl], [Bgl])
            Cst = A.f32(257)
            Cbf = A.bf(258)
            BC, BCb = Buf(), Buf()
            NSET = 3
            sets = [mkset(A, [("qk", "b", 256), ("vaug", "b", 258), ("qkT", "b", 256), ("PT", "b", 128),
                              ("tmpC", "f", 257), ("sm", "f", 8), ("ht", "f", 256), ("og", "f", 256),
                              ("ybf", "b", 256), ("yT", "b", 256)]) for _ in range(NSET)]
            sc = 128.0 ** -0.25
            wctx = {}

            def load_w(hh):
                sa = next_ws()
                wa = wslot[sa][:].rearrange("p (k n) -> p k n", k=8)
                wload(sa, 0, wa[:, :, 0:128], w_in[:, :, hh * 128:(hh + 1) * 128])
                wload(sa, 0, wa[:, :, 128:256], w_in[:, :, 512 + hh * 128:512 + (hh + 1) * 128])
                wload(sa, 1, wa[:, :, 256:512], w_in[:, :, 1024 + hh * 256:1024 + (hh + 1) * 256])
                sb_ = next_ws()
                wb = wslot[sb_][:, 0:2048].rearrange("p (k n) -> p k n", k=8)
                wo = wslot[sb_][:, 2048:4096].rearrange("p (c e) -> p c e", c=2)
                wload(sb_, 0, wb, w_in[:, :, 2048 + hh * 256:2048 + (hh + 1) * 256])
                wload(sb_, 1, wo, W["a_w_out"][0, hh * 256:(hh + 1) * 256, :].rearrange("(c p) e -> p c e", p=128))
                wctx[hh] = (sa, wa, sb_, wb, wo)

            def st0(it):
                hh, c = divmod(it, NT)
                if c == 0:
                    load_w(hh)
                sa, wa, sb_, wb, wo = wctx[hh]
                T = sets[it % NSET]
                p1, Bp1 = proj_tm(c, wa, 0, 512, Bws[sa])
                p2, Bp2 = proj_tm(c, wb, 0, 256, Bws[sb_])
                act(T["qk"], p1[:, 0:256], AF.Copy, [Bp1], [T["Bqk"]], scale=sc)
                act(T["vaug"][:, 0:256], p1[:, 256:512], AF.Copy, [Bp1, Bgl], [T["Bvaug"]], scale=ea3[:, c, hh:hh + 1])
                vcopy(T["vaug"][:, 256:257], ea3[:, c, hh:hh + 1], [Bgl], [T["Bvaug"]])
                act(T["og"], p2[:, 0:256], AF.Sigmoid, [Bp2], [T["Bog"]])
                vtt(T["og"], T["og"], gob[:, hh * 256:(hh + 1) * 256], ALU.mult, [T["Bog"], Bsm], [T["Bog"]])

            def st1(it):
                hh, c = divmod(it, NT)
                sa, wa, sb_, wb, wo = wctx[hh]
                T = sets[it % NSET]
                qk, vaug, qkT, PT, tmpC, sm, ht, og, ybf, yT = [T[k] for k in
                    ("qk", "vaug", "qkT", "PT", "tmpC", "sm", "ht", "og", "ybf", "yT")]
                Bqk, Bva, BqkT, BPT, BtC, Bs, Bht, Bog, Bybf, ByT = [T["B" + k] for k in
                    ("qk", "vaug", "qkT", "PT", "tmpC", "sm", "ht", "og", "ybf", "yT")]
                if c == 0:
                    memset(Cst, 0.0, [BC])
                    memset(Cbf, 0.0, [BCb])
                pb, Bpb = next_psb()
                tr(pb[:, 0:128], qk[:, 0:128], [Bqk], [Bpb])
                tr(pb[:, 128:256], qk[:, 128:256], [Bqk], [Bpb])
                vcopy(qkT, pb[:, 0:256], [Bpb], [BqkT])
                pS, BpS = next_psf()
                mm(pS[:, 0:128], qkT[:, 128:256], qkT[:, 0:128], True, True, [BqkT], [BpS])
                vtt(PT, pS[:, 0:128], tri[:], ALU.mult, [BpS, Bconst], [BPT])
                po, Bpo = next_psf()
                mm(po[:, 0:257], PT, vaug[:, 0:257], True, False, [BPT, Bva], [Bpo])
                mm(po[:, 0:257], qkT[:, 0:128], Cbf[:, 0:257], False, True, [BqkT, BCb], [Bpo])
                pC, BpC = next_psf()
                mm(pC[:, 0:257], qk[:, 128:256], vaug[:, 0:257], True, True, [Bqk, Bva], [BpC])
                vtt(tmpC, pC[:, 0:257], Cst, ALU.add, [BpC, BC], [BtC])
                act(Cst, tmpC, AF.Copy, [BtC, Bgl], [BC], scale=ebt3[:, c, hh:hh + 1])
                act(Cbf[:, 0:257], tmpC, AF.Copy, [BtC, Bgl], [BCb], scale=ebt3[:, c, hh:hh + 1])
                ebc = eb3[:, c, hh:hh + 1]
                act(sm[:, 0:1], po[:, 256:257], AF.Abs, [Bpo, Bgl], [Bs], scale=ebc)
                vts(sm[:, 0:1], sm[:, 0:1], 1.0, None, ALU.max, None, [Bs], [Bs])
                vrecip(sm[:, 1:2], sm[:, 0:1], [Bs], [Bs])
                vtt(sm[:, 2:3], sm[:, 1:2], ebc, ALU.mult, [Bs, Bgl], [Bs])
                act(ht, po[:, 0:256], AF.Copy, [Bpo, Bs], [Bht], scale=sm[:, 2:3])
                act(scr[:, 0:256], ht, AF.Square, [Bht], [Bscr, Bs], accum_out=sm[:, 3:4])

            def st2(it):
                hh, c = divmod(it, NT)
                sa, wa, sb_, wb, wo = wctx[hh]
                T = sets[it % NSET]
                sm, ht, og, ybf, yT = [T[k] for k in ("sm", "ht", "og", "ybf", "yT")]
                Bs, Bht, Bog, Bybf, ByT = [T["B" + k] for k in ("sm", "ht", "og", "ybf", "yT")]
                act(sm[:, 4:5], sm[:, 3:4], AF.Sqrt, [Bs], [Bs], scale=1.0 / 256, bias=EPS)
                vrecip(sm[:, 5:6], sm[:, 4:5], [Bs], [Bs])
                vstt(ybf, ht, sm[:, 5:6], og, ALU.mult, ALU.mult, [Bht, Bs, Bog], [Bybf])
                pb2, Bpb2 = next_psb()
                tr(pb2[:, 0:128], ybf[:, 0:128], [Bybf], [Bpb2])
                tr(pb2[:, 128:256], ybf[:, 128:256], [Bybf], [Bpb2])
                vcopy(yT, pb2[:, 0:256], [Bpb2], [ByT])
                outproj_acc(c, [(yT[:, 0:128], ByT), (yT[:, 128:256], ByT)],
                            lambda cc, nh: wo[:, cc, nh * 512:(nh + 1) * 512], Bws[sb_])

            pipeline(4 * NT, [st0, st1, st2])

        def retention_phase(l):
            A = Arena()
            w_in = W["c_w_in"][0].rearrange("(kc p) n -> p kc n", p=128)
            cos_t = A.f32(NT * 128)
            sin_t = A.f32(NT * 128)
            Btab = Buf()
            mark = A.off
            trig_tables(A, invc[:], 128, cos_t, sin_t, Btab)
            A.off = mark
            cos3 = cos_t.rearrange("p (t i) -> p t i", i=128)
            sin3 = sin_t.rearrange("p (t i) -> p t i", i=128)
            P.fence()
            gobs = [A.f32(512) for _ in range(2)]
            Bgos = [Buf(), Buf()]
            R = [A.f32(512) for _ in range(2)]
            Rb = [A.bf(512) for _ in range(2)]
            BR = [Buf(), Buf()]
            BRb = [Buf(), Buf()]
            sets = [mkset(A, [("ra", "f", 256), ("rb", "f", 256), ("rc", "f", 256), ("rd", "f", 256), ("ro", "f", 512),
                              ("qkb", "b", 512), ("qkT", "b", 512), ("vb", "b", 512), ("PT", "b", 128),
                              ("tmpR0", "f", 512), ("tmpR1", "f", 512), ("sg", "f", 512), ("yn", "f", 512),
                              ("ybf", "b", 512), ("yT", "b", 512), ("st6", "f", 8)]) for _ in range(2)]
            wctx = {}

            def load_w(hh):
                gob, Bgo = gobs[hh % 2], Bgos[hh % 2]
                dma("sp", gob, W["c_g_out"][0, hh].partition_broadcast(128), [], [Bgo], next_io())
                sa = next_ws()
                wa = wslot[sa][:].rearrange("p (k n) -> p k n", k=8)
                wload(sa, 0, wa[:, :, 0:256], w_in[:, :, hh * 256:(hh + 1) * 256])
                wload(sa, 1, wa[:, :, 256:512], w_in[:, :, 1024 + hh * 256:1024 + (hh + 1) * 256])
                sv = next_ws()
                wv = wslot[sv][:].rearrange("p (k n) -> p k n", k=8)
                wload(sv, 0, wv, w_in[:, :, 2048 + hh * 512:2048 + (hh + 1) * 512])
                sg_ = next_ws()
                wgt = wslot[sg_][:].rearrange("p (k n) -> p k n", k=8)
                wload(sg_, 0, wgt, w_in[:, :, 4096 + hh * 512:4096 + (hh + 1) * 512])
                so = next_ws()
                wo = wslot[so][:].rearrange("p (c e) -> p c e", c=4)
                wload(so, 0, wo, W["c_w_out"][0, hh * 512:(hh + 1) * 512, :].rearrange("(c p) e -> p c e", p=128))
                wctx[hh] = (sa, wa, sv, wv, sg_, wgt, so, wo, gob, Bgo)

            def st0(it):
                hh, c = divmod(it, NT)
                sa, wa, sv, wv, sg_, wgt, so, wo, gob, Bgo = wctx[hh]
                T = sets[it % 2]
                ra, rb, rc, rd, vb, sg = [T[k] for k in ("ra", "rb", "rc", "rd", "vb", "sg")]
                Bra, Brb, Brc, Brd, Bvb, Bsg = [T["B" + k] for k in ("ra", "rb", "rc", "rd", "vb", "sg")]
                pqk, Bpqk = proj_tm(c, wa, 0, 512, Bws[sa])
                pv, Bpv = proj_tm(c, wv, 0, 512, Bws[sv])
                pg, Bpg = proj_tm(c, wgt, 0, 512, Bws[sg_])
                x4 = pqk[:].rearrange("p (q h i) -> p q h i", q=2, h=2)
                cb = bc_mid(cos3[:, c, :], 2)
                sb2 = bc_mid(sin3[:, c, :], 2)
                ra3 = ra.rearrange("p (q i) -> p q i", q=2)
                rb3 = rb.rearrange("p (q i) -> p q i", q=2)
                rc3 = rc.rearrange("p (q i) -> p q i", q=2)
                rd3 = rd.rearrange("p (q i) -> p q i", q=2)
                vtt(ra3, x4[:, :, 0, :], cb, ALU.mult, [Bpqk, Btab], [Bra])
                vtt(rb3, x4[:, :, 1, :], sb2, ALU.mult, [Bpqk, Btab], [Brb])
                vtt(rc3, x4[:, :, 1, :], cb, ALU.mult, [Bpqk, Btab], [Brc])
                vtt(rd3, x4[:, :, 0, :], sb2, ALU.mult, [Bpqk, Btab], [Brd])
                act(vb, pv[:], AF.Copy, [Bpv], [Bvb])
                act(sg, pg[:], AF.Silu, [Bpg], [Bsg])

            def st1(it):
                hh, c = divmod(it, NT)
                sa, wa, sv, wv, sg_, wgt, so, wo, gob, Bgo = wctx[hh]
                gch = RET_GCHUNK[hh]
                T = sets[it % 2]
                ra, rb, rc, rd, ro, qkb, qkT, vb, PT, sg, yn, ybf, yT, st6 = [T[k] for k in
                    ("ra", "rb", "rc", "rd", "ro", "qkb", "qkT", "vb", "PT", "sg", "yn", "ybf", "yT", "st6")]
                Bra, Brb, Brc, Brd, Bro, Bqkb, BqkT, Bvb, BPT, Bsg, Byn, Bybf, ByT, Bst = [T["B" + k] for k in
                    ("ra", "rb", "rc", "rd", "ro", "qkb", "qkT", "vb", "PT", "sg", "yn", "ybf", "yT", "st6")]
                if c == 0:
                    for d2 in range(2):
                        memset(R[d2], 0.0, [BR[d2]])
                        memset(Rb[d2], 0.0, [BRb[d2]])
                ra3 = ra.rearrange("p (q i) -> p q i", q=2)
                rb3 = rb.rearrange("p (q i) -> p q i", q=2)
                rc3 = rc.rearrange("p (q i) -> p q i", q=2)
                rd3 = rd.rearrange("p (q i) -> p q i", q=2)
                ro4 = ro.rearrange("p (q h i) -> p q h i", q=2, h=2)
                vtt(ro4[:, :, 0, :], ra3, rb3, ALU.subtract, [Bra, Brb], [Bro])
                vtt(ro4[:, :, 1, :], rc3, rd3, ALU.add, [Brc, Brd], [Bro])
                act(qkb[:, 0:256], ro[:, 0:256], AF.Copy, [Bro, Bconst], [Bqkb], scale=retc[:, hh:hh + 1])
                act(qkb[:, 256:512], ro[:, 256:512], AF.Copy, [Bro, Bconst], [Bqkb], scale=retc[:, 4 + hh:5 + hh])
                pb, Bpb = next_psb()
                for j in range(4):
                    tr(pb[:, j * 128:(j + 1) * 128], qkb[:, j * 128:(j + 1) * 128], [Bqkb], [Bpb])
                vcopy(qkT, pb[:, 0:512], [Bpb], [BqkT])
                pS, BpS = next_psf()
                for d2 in range(2):
                    mm(pS[:, 0:128], qkT[:, 256 + d2 * 128:256 + (d2 + 1) * 128], qkT[:, d2 * 128:(d2 + 1) * 128],
                       d2 == 0, d2 == 1, [BqkT], [BpS])
                vtt(PT, pS[:, 0:128], tri[:], ALU.mult, [BpS, Bconst], [BPT])
                py, Bpy = next_psf()
                mm(py[:], PT, vb, True, False, [BPT, Bvb], [Bpy])
                for d2 in range(2):
                    mm(py[:], qkT[:, d2 * 128:(d2 + 1) * 128], Rb[d2], False, d2 == 1, [BqkT, BRb[d2]], [Bpy])
                for d2 in range(2):
                    tmpR, BtR = T["tmpR%d" % d2], T["BtmpR%d" % d2]
                    pR, BpR = next_psf()
                    mm(pR[:], qkb[:, 256 + d2 * 128:256 + (d2 + 1) * 128], vb, True, True, [Bqkb, Bvb], [BpR])
                    vtt(tmpR, pR[:], R[d2], ALU.add, [BpR, BR[d2]], [BtR])
                    act(R[d2], tmpR, AF.Copy, [BtR], [BR[d2]], scale=gch)
                    act(Rb[d2], tmpR, AF.Copy, [BtR], [BRb[d2]], scale=gch)
                P.add("dve", lambda e, py=py, st6=st6: e.bn_stats(out=st6[:, 0:6], in_=py[:]), reads=[Bpy], writes=[Bst])
                P.add("dve", lambda e, st6=st6: e.bn_aggr(out=st6[:, 6:8], in_=st6[:, 0:6]), reads=[Bst], writes=[Bst])
                act(st6[:, 0:1], st6[:, 7:8], AF.Sqrt, [Bst], [Bst], bias=EPS)
                vrecip(st6[:, 1:2], st6[:, 0:1], [Bst], [Bst])
                vts(yn, py[:], st6[:, 6:7], st6[:, 1:2], ALU.subtract, ALU.mult, [Bpy, Bst], [Byn])
                vtt(sg, sg, gob, ALU.mult, [Bsg, Bgo], [Bsg])
                vtt(ybf, yn, sg, ALU.mult, [Byn, Bsg], [Bybf])
                pb2, Bpb2 = next_psb()
                for j in range(4):
                    tr(pb2[:, j * 128:(j + 1) * 128], ybf[:, j * 128:(j + 1) * 128], [Bybf], [Bpb2])
                vcopy(yT, pb2[:, 0:512], [Bpb2], [ByT])
                outproj_acc(c, [(yT[:, j * 128:(j + 1) * 128], ByT) for j in range(4)],
                            lambda cc, nh: wo[:, cc, nh * 512:(nh + 1) * 512], Bws[so])

            for hh in range(4):
                load_w(hh)
                pipeline(NT, [lambda c, hh=hh: st0(hh * NT + c), lambda c, hh=hh: st1(hh * NT + c)])

        def moba_phase(l):
            A = Arena()
            w_in = W["b_w_in"][0].rearrange("(kc p) n -> p kc n", p=128)
            cos_t = A.f32(NT * 16)
            sin_t = A.f32(NT * 16)
            Btab = Buf()
            mark = A.off
            trig_tables(A, invb[:], 16, cos_t, sin_t, Btab)
            A.off = mark
            cos3 = cos_t.rearrange("p (t i) -> p t i", i=16)
            sin3 = sin_t.rearrange("p (t i) -> p t i", i=16)
            P.fence()
            gqk = A.f32(256)
            Bg = Buf()
            dma("sp", gqk[:, 0:128], W["b_g_q"][0].partition_broadcast(128), [], [Bg], next_io())
            dma("sp", gqk[:, 128:256], W["b_g_k"][0].partition_broadcast(128), [], [Bg], next_io())
            NBLK = S // 256
            hsets = []
            for _ in range(2):
                d = {"qT": A.bf(S), "kT": A.bf(S), "vaug": A.bf(NT * 130), "kmf": A.f32(8), "kmb": A.bf(8)}
                d["BqT"] = [Buf() for _ in range(NT)]
                d["BkT"] = [Buf() for _ in range(NT)]
                d["Bva"] = [Buf() for _ in range(NT)]
                d["Bkm"] = Buf()
                hsets.append(d)
            NPSET = 3
            psets = [mkset(A, [("qkn", "f", 256), ("r4", "f", 128), ("qkb", "b", 256), ("sm", "f", 8)])
                     for _ in range(NPSET)]
            NQS = 4
            qsets = [mkset(A, [("gm", "f", 8), ("m8", "f", 8), ("sel", "f", 8), ("acc", "f", 132), ("obf", "b", 128),
                               ("yTt", "b", 128)]) for _ in range(NQS)]
            NPT = 4
            ptsets = [mkset(A, [("PT", "b", 256)]) for _ in range(NPT)]
            sc = 128.0 ** -0.5
            for hh in range(8):
                H = hsets[hh % 2]
                qT, kT, kmf, kmb = H["qT"], H["kT"], H["kmf"], H["kmb"]
                va3 = H["vaug"].rearrange("p (t e) -> p t e", e=130)
                BqT, BkT, Bva, Bkm = H["BqT"], H["BkT"], H["Bva"], H["Bkm"]
                sa = next_ws()
                wa = wslot[sa][:, 0:3072].rearrange("p (k n) -> p k n", k=8)
                wload(sa, 0, wa[:, :, 0:128], w_in[:, :, hh * 128:(hh + 1) * 128])
                wload(sa, 0, wa[:, :, 128:256], w_in[:, :, 1024 + hh * 128:1024 + (hh + 1) * 128])
                wload(sa, 1, wa[:, :, 256:384], w_in[:, :, 2048 + hh * 128:2048 + (hh + 1) * 128])
                wo = wslot[sa][:, 3072:4096]
                wload(sa, 1, wo, W["b_w_out"][0, hh * 128:(hh + 1) * 128, :])
                pctx = {}

                def ip0(c):
                    T = psets[c % NPSET]
                    qkn, sm = T["qkn"], T["sm"]
                    Bqkn, Bs = T["Bqkn"], T["Bsm"]
                    p1, Bp1 = proj_tm(c, wa, 0, 384, Bws[sa])
                    act(scr[:, 0:128], p1[:, 0:128], AF.Square, [Bp1], [Bscr, Bs], accum_out=sm[:, 0:1])
                    act(scr[:, 128:256], p1[:, 128:256], AF.Square, [Bp1], [Bscr, Bs], accum_out=sm[:, 1:2])
                    act(va3[:, c, 0:128], p1[:, 256:384], AF.Copy, [Bp1], [Bva[c]])
                    act(sm[:, 2:4], sm[:, 0:2], AF.Sqrt, [Bs], [Bs], scale=1.0 / 128, bias=EPS)
                    vrecip(sm[:, 4:6], sm[:, 2:4], [Bs], [Bs])
                    vstt(qkn[:, 0:128], p1[:, 0:128], sm[:, 4:5], gqk[:, 0:128], ALU.mult, ALU.mult,
                         [Bp1, Bs, Bg], [Bqkn])
                    vstt(qkn[:, 128:256], p1[:, 128:256], sm[:, 5:6], gqk[:, 128:256], ALU.mult, ALU.mult,
                         [Bp1, Bs, Bg], [Bqkn])
                    memset(va3[:, c, 128:129], 1.0, [Bva[c]])

                def ip1(c):
                    T = psets[c % NPSET]
                    qkn, r4, qkb = T["qkn"], T["r4"], T["qkb"]
                    Bqkn, Br4, Bqkb = T["Bqkn"], T["Br4"], T["Bqkb"]
                    q3 = qkn.rearrange("p (q d) -> p q d", q=2)
                    x1 = q3[:, :, 0:16]
                    x2 = q3[:, :, 16:32]
                    cb = bc_mid(cos3[:, c, :], 2)
                    sb2 = bc_mid(sin3[:, c, :], 2)
                    r43 = r4.rearrange("p (a q i) -> p a q i", a=4, q=2)
                    vtt(r43[:, 0], x1, cb, ALU.mult, [Bqkn, Btab], [Br4])
                    vtt(r43[:, 1], x2, sb2, ALU.mult, [Bqkn, Btab], [Br4])
                    vtt(r43[:, 2], x2, cb, ALU.mult, [Bqkn, Btab], [Br4])
                    vtt(r43[:, 3], x1, sb2, ALU.mult, [Bqkn, Btab], [Br4])
                    vtt(x1, r43[:, 0], r43[:, 1], ALU.subtract, [Br4], [Bqkn])
                    vtt(x2, r43[:, 2], r43[:, 3], ALU.add, [Br4], [Bqkn])
                    act(qkb[:, 0:128], qkn[:, 0:128], AF.Copy, [Bqkn], [Bqkb], scale=sc)
                    act(qkb[:, 128:256], qkn[:, 128:256], AF.Copy, [Bqkn], [Bqkb])
                    pb, Bpb = next_psb()
                    tr(pb[:, 0:128], qkb[:, 0:128], [Bqkb], [Bpb])
                    tr(pb[:, 128:256], qkb[:, 128:256], [Bqkb], [Bpb])
                    vcopy(qT[:, c * 128:(c + 1) * 128], pb[:, 0:128], [Bpb], [BqT[c]])
                    vcopy(kT[:, c * 128:(c + 1) * 128], pb[:, 128:256], [Bpb], [BkT[c]])

                pipeline(NT, [ip0, ip1])
                P.add("dve", lambda e, kmf=kmf, kT=kT: e.tensor_reduce(
                    out=kmf[:, 0:NBLK], in_=kT.rearrange("p (b n) -> p b n", n=256), axis=AX.X, op=ALU.add),
                      reads=BkT, writes=[Bkm], cost=2300.0)
                act(kmb[:, 0:NBLK], kmf[:, 0:NBLK], AF.Copy, [Bkm], [Bkm], scale=1.0 / 256)
                items = [(qi, b) for qi in range(NT) for b in range(qi // 2 + 1)]
                ictx = {}

                def at0(k):
                    qi, b = items[k]
                    j = qi // 2
                    own = (b == j)
                    chunks = [0] if (own and qi % 2 == 0) else [0, 1]
                    pS, BpS = next_psf()
                    for kc in chunks:
                        kt = b * 2 + kc
                        mm(pS[:, kc * 128:(kc + 1) * 128], kT[:, kt * 128:(kt + 1) * 128],
                           qT[:, qi * 128:(qi + 1) * 128], True, True, [BkT[kt], BqT[qi]], [BpS])
                    PTs = ptsets[k % NPT]
                    PT, BPT = PTs["PT"], PTs["BPT"]
                    n = len(chunks) * 128
                    act(PT[:, 0:n], pS[:, 0:n], AF.Exp, [BpS], [BPT])
                    if own:
                        dg = qi % 2
                        vtt(PT[:, dg * 128:(dg + 1) * 128], PT[:, dg * 128:(dg + 1) * 128], tribf[:], ALU.mult,
                            [BPT, Bconst], [BPT])
                    ictx[k] = chunks

                def at1(k):
                    qi, b = items[k]
                    j = qi // 2
                    nprev = j
                    own = (b == j)
                    chunks = ictx.pop(k)
                    Q = qsets[qi % NQS]
                    gm, m8, sel, acc, obf, yTt = Q["gm"], Q["m8"], Q["sel"], Q["acc"], Q["obf"], Q["yTt"]
                    Bgm, Bsel, Bacc, Bobf, ByT = Q["Bgm"], Q["Bsel"], Q["Bacc"], Q["Bobf"], Q["ByTt"]
                    PTs = ptsets[k % NPT]
                    PT, BPT = PTs["PT"], PTs["BPT"]
                    if b == 0 and nprev > 3:
                        pgt, Bpgt = next_psf()
                        mm(pgt[:, 0:NBLK], qT[:, qi * 128:(qi + 1) * 128], kmb[:, 0:NBLK], True, True,
                           [BqT[qi], Bkm], [Bpgt])
                        memset(gm, -1e30, [Bgm])
                        vcopy(gm[:, 0:nprev], pgt[:, 0:nprev], [Bpgt], [Bgm])
                        P.add("dve", lambda e, m8=m8, gm=gm: e.max(out=m8, in_=gm), reads=[Bgm], writes=[Bgm])
                        vts(sel, gm, m8[:, 2:3], None, ALU.is_ge, None, [Bgm], [Bsel])
                    pO, BpO = next_psf()
                    for ci, kc in enumerate(chunks):
                        kt = b * 2 + kc
                        mm(pO[:, 0:129], PT[:, kc * 128:(kc + 1) * 128], va3[:, kt, 0:129], ci == 0,
                           ci == len(chunks) - 1, [BPT, Bva[kt]], [BpO])
                    use_sel = (not own) and nprev > 3
                    if b == 0:
                        if use_sel:
                            vts(acc[:, 0:129], pO[:, 0:129], sel[:, b:b + 1], None, ALU.mult, None,
                                [BpO, Bsel], [Bacc])
                        else:
                            vcopy(acc[:, 0:129], pO[:, 0:129], [BpO], [Bacc])
                    else:
                        if use_sel:
                            vstt(acc[:, 0:129], pO[:, 0:129], sel[:, b:b + 1], acc[:, 0:129], ALU.mult, ALU.add,
                                 [BpO, Bsel, Bacc], [Bacc])
                        else:
                            vtt(acc[:, 0:129], pO[:, 0:129], acc[:, 0:129], ALU.add, [BpO, Bacc], [Bacc])
                    if own:
                        vrecip(acc[:, 130:131], acc[:, 128:129], [Bacc], [Bacc])
                        act(obf, acc[:, 0:128], AF.Copy, [Bacc], [Bobf], scale=acc[:, 130:131])

                def at2(k):
                    qi, b = items[k]
                    if b != qi // 2:
                        return
                    Q = qsets[qi % NQS]
                    obf, yTt, Bobf, ByT = Q["obf"], Q["yTt"], Q["Bobf"], Q["ByTt"]
                    pb2, Bpb2 = next_psb()
                    tr(pb2[:, 0:128], obf, [Bobf], [Bpb2])
                    vcopy(yTt, pb2[:, 0:128], [Bpb2], [ByT])
                    outproj_acc(qi, [(yTt, ByT)], lambda cc, nh: wo[:, nh * 512:(nh + 1) * 512], Bws[sa])

                def nop_stage(k):
                    pass

                pipeline(len(items), [at0, nop_stage, at1, nop_stage, at2])

        MIXERS = {0: mlstm_phase, 1: moba_phase, 2: retention_phase, 3: rglru_phase}

        _ARENA[0] = True
        for s in range(nseq):
            for t in range(NT):
                dma("sp", h[:, t, :], x_d[s, t * 128:(t + 1) * 128, :], [], [Bh[t]], next_io())
            dma("sp", posi[:], pos_d[s], [], [Bpos], next_io())
            vcopy(posf[:], posi[:], [Bpos], [Bpos])
            for kind, l in plan:
                if kind == "mix":
                    norm_phase(W["norm_mix"][l])
                    P.fence()
                    MIXERS[l](l)
                else:
                    norm_phase(W["norm_ffn"][l])
                    P.fence()
                    ffn_phase(l)
            outs = []
            for t in range(NT):
                outs.append(dma("sp", out_d[s, t * 128:(t + 1) * 128, :], h[:, t, :], [Bh[t]], [], next_io()))
            P.add("sp", None, extra_deps=[sl.last for sl in io_slots])
        _ARENA[0] = False
        P.finalize(sems)
        with nc.Block() as block:
            P.emit(block)
    return nc


def kernel(**inputs):
    ncores = 8
    x = np.ascontiguousarray(inputs["x"], dtype=np.float32)
    pos = np.asarray(inputs["positions"], dtype=np.int32)
    pos = np.ascontiguousarray(pos.reshape(pos.shape[0], -1, 128).transpose(0, 2, 1))
    B = x.shape[0]
    per = B // ncores
    nc = build(nseq=per, S=x.shape[1])
    consts = host_consts()
    in_maps = []
    for c in range(ncores):
        m = {"x": x[c * per:(c + 1) * per], "positions": pos[c * per:(c + 1) * per]}
        for k in DEV_PARAMS:
            m[k] = np.ascontiguousarray(inputs[k], dtype=np.float32)
        m["d_small"] = pack_d_small(inputs)
        m.update(consts)
        in_maps.append(m)
    res = run_bass_kernel_spmd(nc, in_maps, core_ids=list(range(ncores)))
    return np.concatenate([r["out"] for r in res.results], axis=0).astype(np.float32)
```
